# Optimizing a Trainium2 kernel written in Bass

```python
import jax, jax.numpy as jnp
from jax import lax
import numpy as np

D_MODEL = 1024
BATCH = 16
SEQ = 2048
DEPTH = 2

MEM_LEN = 256
N_MIXERS = 2
N_RET_LAYERS = (DEPTH + 1) // 2
N_CONV_LAYERS = DEPTH // 2
MIX_WIDTH = 2 * D_MODEL
XATTN_HEADS = 4
XATTN_WIDTH = MIX_WIDTH // 4
XATTN_HEAD_DIM = XATTN_WIDTH // XATTN_HEADS
BRANCH_WIDTH = MIX_WIDTH - XATTN_WIDTH
RET_HEADS = 8
RET_QK_DIM = D_MODEL // RET_HEADS
RET_V_DIM = BRANCH_WIDTH // RET_HEADS
CHUNK = 128
CONV_WIDTH = 31
ROPE_BASE = 10000.0
EPS = 1e-6
RET_IN_WIDTH = 2 * D_MODEL + BRANCH_WIDTH + XATTN_WIDTH + MIX_WIDTH
CONV_IN_WIDTH = 2 * BRANCH_WIDTH + XATTN_WIDTH + MIX_WIDTH

kernel_name = "hybrid_retention_conformer_memxattn"


def rmsnorm(x, g):
    xf = x.astype(jnp.float32)
    y = xf * lax.rsqrt(jnp.mean(xf * xf, axis=-1, keepdims=True) + EPS)
    return (y * g.astype(jnp.float32)).astype(x.dtype)


def layernorm(x, g, b):
    xf = x.astype(jnp.float32)
    mu = jnp.mean(xf, axis=-1, keepdims=True)
    var = jnp.mean(jnp.square(xf - mu), axis=-1, keepdims=True)
    y = (xf - mu) * lax.rsqrt(var + EPS)
    return (y * g.astype(jnp.float32) + b.astype(jnp.float32)).astype(x.dtype)


def rotary(t, positions):
    half = t.shape[-1] // 2
    inv = ROPE_BASE ** (-jnp.arange(half, dtype=jnp.float32) / half)
    ang = positions.astype(jnp.float32)[..., None] * inv
    cos = jnp.cos(ang)[:, :, None, :]
    sin = jnp.sin(ang)[:, :, None, :]
    tf = t.astype(jnp.float32)
    t1, t2 = tf[..., :half], tf[..., half:]
    out = jnp.concatenate([t1 * cos - t2 * sin, t2 * cos + t1 * sin], axis=-1)
    return out.astype(t.dtype)


def chunkwise_retention(q, k, v):
    b, s, h, dk = q.shape
    dv = v.shape[-1]
    n_chunks = s // CHUNK
    log_gamma = jnp.log1p(-jnp.exp2(-5.0 - jnp.arange(h, dtype=jnp.float32)))
    idx = jnp.arange(CHUNK, dtype=jnp.float32)
    rel = idx[:, None] - idx[None, :]
    intra = jnp.where(rel[None] >= 0, jnp.exp(jnp.maximum(rel, 0.0)[None] * log_gamma[:, None, None]), 0.0)
    xi = jnp.exp((idx[:, None] + 1.0) * log_gamma[None, :])
    zeta = jnp.exp((CHUNK - 1.0 - idx[:, None]) * log_gamma[None, :])
    chunk_decay = jnp.exp(CHUNK * log_gamma)

    def to_chunks(t):
        return jnp.moveaxis(t.reshape(b, n_chunks, CHUNK, h, t.shape[-1]), 1, 0)

    def step(state, qkv):
        qn, kn, vn = qkv
        scores = jnp.einsum('bihd,bjhd->bhij', qn, kn) * intra[None]
        inner = jnp.einsum('bhij,bjhv->bihv', scores, vn)
        cross = jnp.einsum('bihd,bhdv->bihv', qn, state) * xi[None, :, :, None]
        new_state = state * chunk_decay[None, :, None, None] + jnp.einsum(
            'bjhd,bjhv->bhdv', kn * zeta[None, :, :, None], vn)
        return new_state, inner + cross

    state0 = jnp.zeros((b, h, dk, dv), jnp.float32)
    _, ys = lax.scan(step, state0, (to_chunks(q), to_chunks(k), to_chunks(v)))
    return jnp.moveaxis(ys, 0, 1).reshape(b, s, h, dv)


def retention_branch(proj, positions):
    b, s, _ = proj.shape
    q, k, v, xq, gate = jnp.split(
        proj, [D_MODEL, 2 * D_MODEL, 2 * D_MODEL + BRANCH_WIDTH,
               2 * D_MODEL + BRANCH_WIDTH + XATTN_WIDTH], axis=-1)
    q = rotary(q.reshape(b, s, RET_HEADS, RET_QK_DIM), positions)
    k = rotary(k.reshape(b, s, RET_HEADS, RET_QK_DIM), positions) * (RET_QK_DIM ** -0.5)
    v = v.reshape(b, s, RET_HEADS, RET_V_DIM)
    o = chunkwise_retention(q, k, v)
    mu = jnp.mean(o, axis=-1, keepdims=True)
    var = jnp.mean(jnp.square(o - mu), axis=-1, keepdims=True)
    o = ((o - mu) * lax.rsqrt(var + EPS)).reshape(b, s, BRANCH_WIDTH).astype(proj.dtype)
    return o, xq, gate


def conv_branch(proj, dw_w, dw_b, ln_g, ln_b):
    u, g, xq, gate = jnp.split(
        proj, [BRANCH_WIDTH, 2 * BRANCH_WIDTH, 2 * BRANCH_WIDTH + XATTN_WIDTH], axis=-1)
    glu = u * jax.nn.sigmoid(g)
    y = lax.conv_general_dilated(
        glu, dw_w[:, None, :].astype(glu.dtype), window_strides=(1,),
        padding=[(CONV_WIDTH - 1, 0)], dimension_numbers=('NWC', 'WIO', 'NWC'),
        feature_group_count=BRANCH_WIDTH) + dw_b.astype(glu.dtype)
    y = jax.nn.silu(layernorm(y, ln_g, ln_b))
    return y, xq, gate


def memory_attention(xq, mem_k, mem_v):
    b, s, _ = xq.shape
    q = xq.reshape(b, s, XATTN_HEADS, XATTN_HEAD_DIM)
    scores = jnp.einsum('bshd,bmhd->bhsm', q, mem_k).astype(jnp.float32) * (XATTN_HEAD_DIM ** -0.5)
    p = jax.nn.softmax(scores, axis=-1).astype(mem_v.dtype)
    o = jnp.einsum('bhsm,bmhd->bshd', p, mem_v)
    return o.reshape(b, s, XATTN_WIDTH)


def setup_inputs(seed: int = 0) -> dict:
    key = jax.random.key(seed)
    ks = jax.random.split(key, 20)
    f32 = jnp.float32
    nrm = lambda k, shape, scale: jax.random.normal(k, shape, f32) * scale
    x = jax.random.normal(ks[0], (BATCH, SEQ, D_MODEL), f32)
    mem = jax.random.normal(ks[1], (BATCH, MEM_LEN, D_MODEL), f32)
    offset = jax.random.randint(ks[2], (BATCH, 1), 0, 1024, dtype=jnp.int32)
    positions = offset + jnp.arange(SEQ, dtype=jnp.int32)[None, :]
    return {
        "x": x,
        "mem": mem,
        "positions": positions,
        "mem_norm_g": 1.0 + nrm(ks[3], (D_MODEL,), 0.02),
        "w_mem_kv": nrm(ks[4], (D_MODEL, 2 * XATTN_WIDTH), D_MODEL ** -0.5),
        "norm_pre_g": 1.0 + nrm(ks[5], (DEPTH, D_MODEL), 0.02),
        "norm_post_g": 1.0 + nrm(ks[6], (DEPTH, D_MODEL), 0.02),
        "ret_w_in": nrm(ks[7], (N_RET_LAYERS, D_MODEL, RET_IN_WIDTH), D_MODEL ** -0.5),
        "ret_w_out": nrm(ks[8], (N_RET_LAYERS, MIX_WIDTH, D_MODEL), MIX_WIDTH ** -0.5),
        "conv_w_in": nrm(ks[9], (N_CONV_LAYERS, D_MODEL, CONV_IN_WIDTH), D_MODEL ** -0.5),
        "conv_dw_w": nrm(ks[10], (N_CONV_LAYERS, CONV_WIDTH, BRANCH_WIDTH), CONV_WIDTH ** -0.5),
        "conv_dw_b": nrm(ks[11], (N_CONV_LAYERS, BRANCH_WIDTH), 0.02),
        "conv_ln_g": 1.0 + nrm(ks[12], (N_CONV_LAYERS, BRANCH_WIDTH), 0.02),
        "conv_ln_b": nrm(ks[13], (N_CONV_LAYERS, BRANCH_WIDTH), 0.02),
        "conv_w_out": nrm(ks[14], (N_CONV_LAYERS, MIX_WIDTH, D_MODEL), MIX_WIDTH ** -0.5),
    }


def reference(x, mem, positions, mem_norm_g, w_mem_kv, norm_pre_g, norm_post_g,
              ret_w_in, ret_w_out, conv_w_in, conv_dw_w, conv_dw_b, conv_ln_g,
              conv_ln_b, conv_w_out):
    b = x.shape[0]
    mem_kv = rmsnorm(mem, mem_norm_g) @ w_mem_kv
    mem_k, mem_v = jnp.split(mem_kv, 2, axis=-1)
    mem_k = mem_k.reshape(b, MEM_LEN, XATTN_HEADS, XATTN_HEAD_DIM)
    mem_v = mem_v.reshape(b, MEM_LEN, XATTN_HEADS, XATTN_HEAD_DIM)

    for i in range(DEPTH):
        h = rmsnorm(x, norm_pre_g[i])
        j = i // N_MIXERS
        if i % N_MIXERS == 0:
            branch, xq, gate = retention_branch(h @ ret_w_in[j], positions)
            w_out = ret_w_out[j]
        else:
            branch, xq, gate = conv_branch(h @ conv_w_in[j], conv_dw_w[j], conv_dw_b[j],
                                           conv_ln_g[j], conv_ln_b[j])
            w_out = conv_w_out[j]
        xa = memory_attention(xq, mem_k, mem_v)
        y = jnp.concatenate([branch, xa], axis=-1) * jax.nn.silu(gate)
        x = x + rmsnorm(y @ w_out, norm_post_g[i])
    return x
```

```python
import math
import re
import numpy as np
from contextlib import ExitStack
import concourse.bass as bass
import concourse.mybir as mybir
from concourse.bass_utils import run_bass_kernel_spmd

F32 = mybir.dt.float32
BF16 = mybir.dt.bfloat16
I32 = mybir.dt.int32
AF = mybir.ActivationFunctionType
ALU = mybir.AluOpType
AX = mybir.AxisListType

D = 1024
MEM = 256
EPS = 1e-6
NH = 8
DV = 192
BW = 1536
CW = 31
NCH = 4
T = NCH * 128
RET_IN = 6144
CONV_IN = 5632
MAGIC = 12582912.0
DBG_STAGE = 99
SKIP = set()
TWO_PI = 2.0 * math.pi


PS_RE = re.compile(r"^ps\d$")
ARENA_RE = re.compile(r"^(diag|Q\d|K\d|V\d|VAR|qT|kT|ST\d|ON|t1_|t2_|Wo|otmp|conv|sig|yb|ysq|MEAN|RSTD|NMR|z\d|a\d)")


class Prog:
    ENGS = ("pe", "dve", "act", "pool", "sp")

    def __init__(self, nc, stack):
        self.nc = nc
        self.stack = stack
        self.ops = []
        self.last_w = {}
        self.readers = {}

    def sb(self, name, shape, dtype):
        return self.stack.enter_context(self.nc.sbuf_tensor(name, list(shape), dtype))

    def ps(self, name, shape, dtype):
        return self.stack.enter_context(self.nc.psum_tensor(name, list(shape), dtype))

    def op(self, eng, fn, reads=(), writes=(), dma=None, ndma=1):
        idx = len(self.ops)
        deps = set()
        is_dma = dma is not None
        reads = list(reads)
        writes = list(writes)
        if "ar_all" not in writes and any(ARENA_RE.match(k) for k in reads + writes):
            reads.append("ar_all")
        for k in reads:
            if PS_RE.match(k) and k not in writes:
                writes.append(k)

        def add(d, raw):
            if d is None or d == idx:
                return
            od = self.ops[d]
            deps.add(d)
        for r in reads:
            add(self.last_w.get(r), True)
        for w in writes:
            add(self.last_w.get(w), False)
            for rd in self.readers.get(w, ()):
                add(rd, False)
        for r in reads:
            self.readers.setdefault(r, []).append(idx)
        for w in writes:
            self.last_w[w] = idx
            self.readers[w] = []
        import sys as _sys
        fr = _sys._getframe(1)
        while fr.f_code.co_name in ('op', 'pe', 'dve', 'act', 'pool', 'dma', 'mm_group'):
            fr = fr.f_back
        self.ops.append(dict(eng=eng, fn=fn, deps=deps, dma=dma, ndma=ndma, line=fr.f_lineno))
        return idx

    def pe(self, fn, reads=(), writes=()):
        return self.op("pe", fn, reads, writes)

    def dve(self, fn, reads=(), writes=()):
        return self.op("dve", fn, reads, writes)

    def act(self, fn, reads=(), writes=()):
        return self.op("act", fn, reads, writes)

    def pool(self, fn, reads=(), writes=()):
        return self.op("pool", fn, reads, writes)

    def dma(self, queue, semname, fn, reads=(), writes=(), ndma=1):
        return self.op(queue, fn, reads, writes, dma=semname, ndma=ndma)

    def finish(self, final_wait_eng="sp"):
        nc = self.nc
        ops = self.ops
        n = len(ops)
        needed = set()
        for o in ops:
            needed |= o["deps"]
        sem_names = set(self.ENGS)
        for o in ops:
            if o["dma"] is not None:
                sem_names.add("dma:" + o["dma"])
        sems = {}
        for s in sorted(sem_names):
            sems[s] = self.stack.enter_context(nc.semaphore(s.replace(":", "_")))
        counts = {s: 0 for s in sem_names}
        ev = [None] * n
        vc = [None] * n
        clock = {e: {} for e in self.ENGS}
        streams = {e: [] for e in self.ENGS}
        outstanding = {}
        for i, o in enumerate(ops):
            e = o["eng"]
            ck = clock[e]
            waits = {}
            for d in sorted(o["deps"]):
                s, v = ev[d]
                if ck.get(s, 0) >= v:
                    continue
                if waits.get(s, 0) < v:
                    waits[s] = v
                for s2, v2 in vc[d].items():
                    if ck.get(s2, 0) < v2:
                        ck[s2] = v2
            if o["dma"] is not None:
                s = "dma:" + o["dma"]
                counts[s] += 16 * o["ndma"]
                ev[i] = (s, counts[s])
                inc = (s, 16)
                outstanding[s] = counts[s]
                v = dict(ck)
                v[s] = counts[s]
                vc[i] = v
            elif i in needed:
                counts[e] += 1
                ev[i] = (e, counts[e])
                inc = (e, 1)
                v = dict(ck)
                v[e] = counts[e]
                vc[i] = v
            else:
                inc = None
            streams[e].append((waits, o["fn"], inc))
            o["waits"] = dict(waits)
            o["ev"] = ev[i]
        self.sem_counts = counts
        engobj = {"pe": "tensor", "dve": "vector", "act": "scalar", "pool": "gpsimd", "sp": "sync"}
        with nc.Block() as block:
            for e in self.ENGS:
                stream = streams[e]
                fin = outstanding if e == final_wait_eng else None

                def body(eng, stream=stream, fin=fin):
                    for waits, fn, inc in stream:
                        for s, v in waits.items():
                            eng.wait_ge(sems[s], v)
                        ins = fn(eng)
                        if inc is not None:
                            if isinstance(ins, (list, tuple)):
                                for x in ins:
                                    x.then_inc(sems[inc[0]], inc[1])
                            else:
                                ins.then_inc(sems[inc[0]], inc[1])
                    if fin:
                        for s, v in fin.items():
                            eng.wait_ge(sems[s], v)
                getattr(block, engobj[e])(body)


def _gammas():
    h = np.arange(NH, dtype=np.float64)
    return 1.0 - np.exp2(-5.0 - h)


def make_consts():
    g = _gammas()
    idx = np.arange(128, dtype=np.float64)
    c = {}
    c["c_ident"] = np.eye(128, dtype=np.float32)
    c["c_ones"] = np.ones((128, 128), dtype=np.float32)
    half = 64
    inv = (10000.0 ** (-(np.arange(half, dtype=np.float32) / np.float32(half)))).astype(np.float32)
    c["c_invf"] = np.ascontiguousarray(np.broadcast_to(inv[None, :], (128, half))).astype(np.float32)
    causal = (idx[None, :] >= idx[:, None]).astype(np.float64)
    mask = causal[:, None, :] * (g ** -128.0)[None, :, None]
    c["c_mask"] = mask.astype(np.float32)
    xi = g[:, None] ** (idx[None, :] + 1.0)
    c["c_xi"] = np.ascontiguousarray(np.broadcast_to(xi[None], (128, NH, 128))).astype(np.float32)
    zs = (g[None, :] ** (127.0 - idx[:, None])) * (128.0 ** -0.5)
    c["c_zs"] = zs.astype(np.float32)
    return c, [float(x) for x in (g ** 128.0)]


def build_program(nseq, seqlen, layers=(0, 1)):
    ngs = seqlen // T
    ng = nseq * ngs
    ntok = nseq * seqlen
    ncht = ntok // 128
    consts, cdec = make_consts()
    nc = bass.Bass("TRN2", target_bir_lowering=False)

    def din(name, shape, dt=F32):
        return nc.dram_tensor(name, list(shape), dt, kind="ExternalInput").ap()
    x_d = din("x", [ntok, D])
    mem_d = din("mem", [nseq * MEM, D])
    pos_d = din("pos", [128, ncht], I32)
    wkv_d = din("w_mem_kv", [D, D])
    w_in_d = [din("ret_w_in", [D, RET_IN]), din("conv_w_in", [D, CONV_IN])]
    w_out_d = [din("ret_w_out", [2048, D]), din("conv_w_out", [2048, D])]
    gmem_d = din("g_mem", [128, 8])
    gpre_d = din("g_pre", [128, 2, 8])
    gpost_d = din("g_post", [128, 2, D])
    dw_d = din("dw_w", [128, 12, CW])
    dwb_d = din("dw_b", [128, 12])
    lng_d = din("ln_g", [128, 12])
    lnb_d = din("ln_b", [128, 12])
    cd = {k: din(k, v.shape) for k, v in consts.items()}
    out_d = nc.dram_tensor("out", [ntok, D], F32, kind="ExternalOutput").ap()

    wkv_s = nc.dram_tensor("wkv_s", [2, 128, 8, 512], BF16).ap()
    win_s = [nc.dram_tensor("win0_s", [12, 128, 8, 512], BF16).ap(),
             nc.dram_tensor("win1_s", [11, 128, 8, 512], BF16).ap()]
    wout_s = [nc.dram_tensor("wout0_s", [2, 128, 16, 512], BF16).ap(),
              nc.dram_tensor("wout1_s", [2, 128, 16, 512], BF16).ap()]
    rope_s = nc.dram_tensor("rope_s", [3, 128, ncht, 64], F32).ap()

    with ExitStack() as st:
        P = Prog(nc, st)
        X = P.sb("X", [128, NCH, D], F32)
        hb = [P.sb(f"hb{i}", [128, D], BF16) for i in range(2)]
        junk = P.sb("junk", [128, D], BF16)
        hT = P.sb("hT", [128, 8, T], BF16)
        WS = [P.sb(f"WS{i}", [128, 8, 512], BF16) for i in range(3)]
        ARENA_B = 56 * 1024
        arena = P.sb("arena", [128, ARENA_B // 2], BF16)
        arena32 = arena.bitcast(F32)

        def a16(off_bytes, shape):
            n = int(np.prod(shape))
            ap = arena[:, off_bytes // 2: off_bytes // 2 + n]
            return ap

        def a32(off_bytes, shape):
            n = int(np.prod(shape))
            return arena32[:, off_bytes // 4: off_bytes // 4 + n]
        KB = 1024
        Qb = a16(0, [NCH * D]).rearrange("p (c f) -> p c f", c=NCH)
        Kb = a16(8 * KB, [NCH * D]).rearrange("p (c f) -> p c f", c=NCH)
        Vb = a16(16 * KB, [NCH * BW]).rearrange("p (c f) -> p c f", c=NCH)
        qT = [a16(28 * KB + i * 2 * KB, [NH * 128]).rearrange("p (h t) -> p h t", h=NH) for i in range(2)]
        kT = [a16(32 * KB + i * 2 * KB, [NH * 128]).rearrange("p (h t) -> p h t", h=NH) for i in range(2)]
        STb = [a16(36 * KB + i * 1 * KB, [4 * 128]).rearrange("p (h t) -> p h t", h=4) for i in range(4)]
        ONb = [a16(40 * KB + i * 3 * KB, [BW]) for i in range(2)]
        t1b = [a32(46 * KB + i * 2 * KB, [512]) for i in range(2)]
        t2b = [a32(50 * KB + i * 2 * KB, [512]) for i in range(2)]
        Wo = [a16(i * 16 * KB, [16 * 512]).rearrange("p (k n) -> p k n", k=16) for i in range(2)]
        otmp = a32(32 * KB, [D])
        convT = a32(0, [12 * T]).rearrange("p (c t) -> p c t", c=12)
        sigb = [a16(24 * KB + i * KB, [T]) for i in range(2)]
        ybb = [a16(26 * KB + i * KB, [T]) for i in range(2)]
        ysqb = [a16(28 * KB + i * KB, [T]) for i in range(2)]
        MEAN = a32(30 * KB, [T])
        RSTD = a32(32 * KB, [T])
        NMR = a32(34 * KB, [T])
        VAR = a32(36 * KB, [T])
        zb = [a32(38 * KB + i * 2 * KB, [T]) for i in range(2)]
        ab = [a16(42 * KB + i * KB, [T]) for i in range(2)]
        diagA = a16(44 * KB, [16 * 128]).rearrange("p (j c) -> p j c", j=16)
        diagB2 = [a16((48 + 4 * i) * KB, [16 * 128]).rearrange("p (j c) -> p j c", j=16) for i in range(2)]
        ARENA_KEYS = []

        xqT = P.sb("xqT", [128, 4, T], BF16)
        yT = P.sb("yT", [128, 16, T], BF16)
        pT = [P.sb(f"pT{i}", [128, T], BF16) for i in range(4)]
        rinv = P.sb("rinv", [128, T], F32)
        ontT = P.sb("ontT", [128, 12, 128], BF16)
        xtmp = P.sb("xtmp", [128, T], F32)
        rinv2 = [rinv, P.sb("rinv_b", [128, T], F32)]
        xtmp2 = [xtmp, P.sb("xtmp_b", [128, T], F32)]
        state = P.sb("state", [128, NH, DV], F32)
        state_bf = P.sb("state_bf", [128, NH, DV], BF16)
        gluT = P.sb("gluT", [128, 12, T + 32], BF16)
        ident = P.sb("ident", [128, 128], BF16)
        ones = P.sb("ones", [128, 128], BF16)
        identf = P.sb("identf", [128, 128], F32)
        invf = P.sb("invf", [128, 64], F32)
        mask = P.sb("mask", [128, NH, 128], F32)
        xi = P.sb("xi", [128, NH, 128], F32)
        zs = P.sb("zs", [128, NH], F32)
        gmem = P.sb("gmem", [128, 8], F32)
        gpre = P.sb("gpre", [128, 2, 8], F32)
        gpost = P.sb("gpost", [128, 2, D], F32)
        dw = P.sb("dw", [128, 12, CW], F32)
        dwb = P.sb("dwb", [128, 12], F32)
        lng = P.sb("lng", [128, 12], F32)
        lnb = P.sb("lnb", [128, 12], F32)
        mhalf = P.sb("mhalf", [128, T], F32)
        rope = P.sb("rope", [128, 3, NCH, 64], F32)
        posi = P.sb("posi", [128, ncht], I32)
        posf = P.sb("posf", [128, ncht], F32)
        rtmp = [P.sb(f"rtmp{i}", [128, NCH, 64], F32) for i in range(3)]
        memkT = P.sb("memkT", [128, 4, MEM], BF16)
        memv = P.sb("memv", [128, 2, 512], BF16)
        small = P.sb("small", [128, 512], F32)
        fdummy = P.sb("fdummy", [128, 1], F32)
        psum = P.ps("psum", [128, 8, 512], F32)
        psum16 = psum.bitcast(BF16)

        def bank(i):
            return psum[:, i, :]

        def bank16(i):
            return psum16[:, i, :]

        cnt = {"stat": 0}

        def stat(n):
            o = cnt["stat"]
            cnt["stat"] = (o + 1) % 128
            return small[:, 4 * o:4 * o + n], f"small{o}"

        def ld(q, sem, dst, src, key):
            P.dma(q, sem, lambda e: e.dma_start(out=dst, in_=src), writes=[key])
        ld("pool", "c0", ident[:], cd["c_ident"], "ident")
        ld("pool", "c1", ones[:], cd["c_ones"], "ones")
        ld("sp", "c1f", identf[:], cd["c_ident"], "identf")
        ld("sp", "c2", invf[:], cd["c_invf"], "invf")
        ld("sp", "c3", mask[:], cd["c_mask"], "mask")
        ld("sp", "c4", xi[:], cd["c_xi"], "xi")
        ld("sp", "c5", zs[:], cd["c_zs"], "zs")
        ld("sp", "c6", gmem[:], gmem_d, "gmem")
        ld("sp", "c7", gpre[:], gpre_d, "gpre")
        ld("sp", "c8", gpost[:], gpost_d, "gpost")
        ld("sp", "c9", dw[:], dw_d, "dw")
        ld("sp", "c10", dwb[:], dwb_d, "dwb")
        ld("sp", "c11", lng[:], lng_d, "lng")
        ld("sp", "c12", lnb[:], lnb_d, "lnb")
        ld("sp", "c13", posi[:], pos_d, "posi")
        P.pool(lambda e: e.memset(mhalf[:], -0.5), writes=["mhalf"])

        def cast_in(sem, dst, src_w, c0, key):
            P.dma("pool", sem, lambda e: e.dma_start(
                out=dst, in_=src_w.rearrange("(k p) n -> p k n", p=128)[:, :, c0:c0 + 512]), writes=[key])

        def cast_out(sem, dst, src_w, c0, key):
            P.dma("pool", sem, lambda e: e.dma_start(
                out=dst, in_=src_w.rearrange("(k p) n -> p k n", p=128)[:, :, c0:c0 + 512]), writes=[key])
        for b in range(2):
            cast_in(f"ck{b}", wkv_s[b], wkv_d, b * 512, f"wkv_s{b}")
        L0_ORDER = list(range(12))
        L1_ORDER = [0, 3, 1, 4, 2, 5, 6, 7, 8, 9, 10]
        if 0 in layers:
            for b in L0_ORDER:
                cast_in(f"cw0_{b}", win_s[0][b], w_in_d[0], b * 512, f"win0_{b}")
            for b in range(2):
                cast_out(f"co0_{b}", wout_s[0][b], w_out_d[0], b * 512, f"wout0_{b}")
        if 1 in layers:
            for b in L1_ORDER:
                cast_in(f"cw1_{b}", win_s[1][b], w_in_d[1], b * 512, f"win1_{b}")
            for b in range(2):
                cast_out(f"co1_{b}", wout_s[1][b], w_out_d[1], b * 512, f"wout1_{b}")

        wlist = []
        for s in range(nseq):
            wlist += [(wkv_s[0], "wkv_s0"), (wkv_s[1], "wkv_s1")]
            for g in range(ngs):
                if 0 in layers:
                    wlist += [(win_s[0][b], f"win0_{b}") for b in L0_ORDER]
                if 1 in layers:
                    wlist += [(win_s[1][b], f"win1_{b}") for b in L1_ORDER]
        wstate = {"next_load": 0, "next_use": 0}

        def w_issue():
            i = wstate["next_load"]
            if i >= len(wlist):
                return
            src, key = wlist[i]
            sl = i % 3
            P.dma("sp", f"ws{sl}", lambda e: e.dma_start(out=WS[sl][:], in_=src), reads=[key], writes=[f"WS{sl}"])
            wstate["next_load"] = i + 1

        def w_next():
            i = wstate["next_use"]
            wstate["next_use"] = i + 1
            return WS[i % 3], f"WS{i % 3}"
        for _ in range(3):
            w_issue()

        def fence():
            P.pool(lambda e: e.memset(fdummy[:], 0.0), writes=["ar_all", "fdummy"])

        P.dve(lambda e: e.tensor_copy(out=posf[:], in_=posi[:]), reads=["posi"], writes=["posf"])
        for g in range(ng):
            ang, red, r = rtmp
            P.dve(lambda e, g=g: e.tensor_tensor(
                out=ang[:], in0=posf[:, g * NCH:(g + 1) * NCH].unsqueeze(2).broadcast_to([128, NCH, 64]),
                in1=invf[:].unsqueeze(1).broadcast_to([128, NCH, 64]), op=ALU.mult),
                reads=["posf", "invf"], writes=["rt0"])
            for which, shift in ((1, 0.0), (0, math.pi / 2)):
                def f_red(e, shift=shift):
                    e.tensor_scalar(out=red[:], in0=ang[:], scalar1=shift, scalar2=1.0 / TWO_PI,
                                    op0=ALU.add, op1=ALU.mult)
                    e.tensor_scalar(out=red[:], in0=red[:], scalar1=MAGIC, scalar2=None, op0=ALU.add)
                    return e.tensor_scalar(out=red[:], in0=red[:], scalar1=-MAGIC, scalar2=-TWO_PI,
                                           op0=ALU.add, op1=ALU.mult)
                P.dve(lambda e, shift=shift: e.tensor_scalar(
                    out=red[:], in0=ang[:], scalar1=shift, scalar2=1.0 / TWO_PI, op0=ALU.add, op1=ALU.mult),
                    reads=["rt0"], writes=["rt1"])
                P.dve(lambda e: e.tensor_scalar(out=r[:], in0=red[:], scalar1=MAGIC, scalar2=None, op0=ALU.add),
                      reads=["rt1"], writes=["rt2"])
                P.dve(lambda e: e.tensor_scalar(out=red[:], in0=r[:], scalar1=-MAGIC, scalar2=-TWO_PI,
                                                op0=ALU.add, op1=ALU.mult),
                      reads=["rt2"], writes=["rt1"])
                P.dve(lambda e, shift=shift: e.scalar_tensor_tensor(
                    out=r[:], in0=ang[:], scalar=shift, in1=red[:], op0=ALU.add, op1=ALU.add),
                    reads=["rt0", "rt1"], writes=["rt2"])
                P.dve(lambda e: e.tensor_scalar(out=red[:], in0=r[:], scalar1=3.14159, scalar2=-3.14159,
                                                op0=ALU.min, op1=ALU.max),
                      reads=["rt2"], writes=["rt1"])
                P.act(lambda e, which=which: e.activation(out=rope[:, which], in_=red[:], func=AF.Sin),
                      reads=["rt1"], writes=[f"rope{which}"])
            P.act(lambda e: e.mul(out=rope[:, 2], in_=rope[:, 1], mul=-1.0), reads=["rope1"], writes=["rope2"])
            P.dma("sp", "ropest", lambda e, g=g: e.dma_start(
                out=rope_s[:, :, g * NCH:(g + 1) * NCH, :].rearrange("w p c d -> p w c d"), in_=rope[:]),
                reads=["rope0", "rope1", "rope2"], writes=[f"rope_s{g}"])

        def norm_transpose(src_ap, src_key, gsc_ap, gsc_key, dst_col0, tr_bank, hbi):
            ss, kss = stat(1)
            rs, krs = stat(1)
            hbt = hb[hbi]
            P.act(lambda e: e.activation(out=junk[:], in_=src_ap, func=AF.Square, accum_out=ss),
                  reads=[src_key], writes=["junk", kss])
            P.dve(lambda e: e.tensor_scalar(out=rs, in0=ss, scalar1=1.0 / D, scalar2=EPS, op0=ALU.mult, op1=ALU.add),
                  reads=[kss], writes=[krs])
            P.pool(lambda e: e.tensor_tensor(out=rs, in0=rs, in1=mhalf[:, 0:1], op=ALU.pow),
                   reads=[krs, "mhalf"], writes=[krs])
            P.act(lambda e: e.activation(out=hbt[:], in_=src_ap, func=AF.Copy, scale=rs),
                  reads=[src_key, krs], writes=[f"hb{hbi}"])
            pb = bank16(tr_bank)

            def ftr(e):
                ins = None
                for k in range(8):
                    ins = e.transpose(out=pb[:, k * 128:(k + 1) * 128], in_=hbt[:, k * 128:(k + 1) * 128],
                                      identity=ident[:])
                return ins
            P.pe(ftr, reads=[f"hb{hbi}", "ident"], writes=[f"ps{tr_bank}"])
            P.dve(lambda e: e.tensor_tensor(
                out=hT[:, :, dst_col0:dst_col0 + 128], in0=pb.rearrange("p (k t) -> p k t", k=8),
                in1=gsc_ap.unsqueeze(2).broadcast_to([128, 8, 128]), op=ALU.mult),
                reads=[f"ps{tr_bank}", gsc_key], writes=[f"hT{dst_col0 // 128}"])

        def norm_transpose_group(L):
            ss4, kss = stat(4)
            rs4, krs = stat(4)
            for c in range(NCH):
                P.act(lambda e, c=c: e.activation(out=junk[:], in_=X[:, c, :], func=AF.Square,
                                                  accum_out=ss4[:, c:c + 1]),
                      reads=[f"X{c}"], writes=["junk", kss + f"_{c}"])
            P.dve(lambda e: e.tensor_scalar(out=rs4, in0=ss4, scalar1=1.0 / D, scalar2=EPS, op0=ALU.mult, op1=ALU.add),
                  reads=[kss + f"_{c}" for c in range(NCH)], writes=[krs])
            P.pool(lambda e: e.tensor_tensor(out=rs4, in0=rs4, in1=mhalf[:, 0:4], op=ALU.pow),
                   reads=[krs, "mhalf"], writes=[krs])
            for c in range(NCH):
                hbi = c % 2
                hbt = hb[hbi]
                tr_bank = 2 + (c % 2)
                P.act(lambda e, c=c, hbt=hbt: e.activation(out=hbt[:], in_=X[:, c, :], func=AF.Copy,
                                                         scale=rs4[:, c:c + 1]),
                      reads=[f"X{c}", krs], writes=[f"hb{hbi}"])
                pb = bank16(tr_bank)

                def ftr(e, hbt=hbt, pb=pb):
                    ins = None
                    for k in range(8):
                        ins = e.transpose(out=pb[:, k * 128:(k + 1) * 128], in_=hbt[:, k * 128:(k + 1) * 128],
                                          identity=ident[:])
                    return ins
                P.pe(ftr, reads=[f"hb{hbi}", "ident"], writes=[f"ps{tr_bank}"])
                P.dve(lambda e, c=c, pb=pb: e.tensor_tensor(
                    out=hT[:, :, c * 128:(c + 1) * 128], in0=pb.rearrange("p (k t) -> p k t", k=8),
                    in1=gpre[:, L, :].unsqueeze(2).broadcast_to([128, 8, 128]), op=ALU.mult),
                    reads=[f"ps{tr_bank}", "gpre"], writes=[f"hT{c}"])

        def mm_group(out_ap, pairs, reads, wkey):
            def f(e):
                ins = None
                n = len(pairs)
                for i, (l, r) in enumerate(pairs):
                    ins = e.matmul(out_ap, lhsT=l, rhs=r, start=(i == 0), stop=(i == n - 1))
                return ins
            P.pe(f, reads=reads, writes=[wkey])

        hT_keys = [f"hT{c}" for c in range(NCH)]

        def mem_kv(s):
            for mc in range(2):
                P.dma("sp", f"xl{mc}", lambda e, mc=mc: e.dma_start(
                    out=X[:, mc, :], in_=mem_d[s * MEM + mc * 128: s * MEM + (mc + 1) * 128, :]),
                    writes=[f"X{mc}"])
            for mc in range(2):
                norm_transpose(X[:, mc, :], f"X{mc}", gmem[:], "gmem", mc * 128, mc, mc)
            w0, k0 = w_next()
            for h in range(4):
                bk = 2 + (h % 2)
                mm_group(bank(bk)[:, 0:MEM],
                         [(w0[:, k, h * 128:(h + 1) * 128], hT[:, k, 0:MEM]) for k in range(8)],
                         [k0, "hT0", "hT1"], f"ps{bk}")
                P.dve(lambda e, h=h, bk=bk: e.tensor_copy(out=memkT[:, h, :], in_=bank(bk)[:, 0:MEM]),
                      reads=[f"ps{bk}"], writes=["memkT"])
            w_issue()
            w1, k1 = w_next()
            for mc in range(2):
                bk = 4 + mc
                mm_group(bank(bk), [(hT[:, k, mc * 128:(mc + 1) * 128], w1[:, k, :]) for k in range(8)],
                         [k1, f"hT{mc}"], f"ps{bk}")
                P.act(lambda e, mc=mc, bk=bk: e.activation(out=memv[:, mc, :], in_=bank(bk), func=AF.Copy),
                      reads=[f"ps{bk}"], writes=["memv"])
            w_issue()

        def mem_attn(heads=(0, 1, 2, 3)):
            sc = 128.0 ** -0.5
            for hd in heads:
                par = hd % 2
                b0 = 4 * par
                pts = [pT[2 * par + mc] for mc in range(2)]
                ptk = [f"pT{2 * par + mc}" for mc in range(2)]
                ri, xt = rinv2[par], xtmp2[par]
                for mc in range(2):
                    bk = b0 + mc
                    mm_group(bank(bk), [(memkT[:, hd, mc * 128:(mc + 1) * 128], xqT[:, hd, :])],
                             ["memkT", "xqT"], f"ps{bk}")
                    P.act(lambda e, mc=mc, bk=bk, pts=pts: e.activation(out=pts[mc][:], in_=bank(bk), func=AF.Exp, scale=sc),
                          reads=[f"ps{bk}"], writes=[ptk[mc]])
                mm_group(bank(b0 + 2), [(memv[:, mc, hd * 128:(hd + 1) * 128], pts[mc][:]) for mc in range(2)],
                         ["memv"] + ptk, f"ps{b0 + 2}")
                mm_group(bank(b0 + 3), [(ones[:], pts[mc][:]) for mc in range(2)], ["ones"] + ptk, f"ps{b0 + 3}")
                P.dve(lambda e, ri=ri, b0=b0: e.reciprocal(out=ri[:], in_=bank(b0 + 3)), reads=[f"ps{b0 + 3}"],
                      writes=[f"rinv{par}"])
                P.dve(lambda e, ri=ri, xt=xt, b0=b0: e.tensor_tensor(out=xt[:], in0=bank(b0 + 2), in1=ri[:], op=ALU.mult),
                      reads=[f"ps{b0 + 2}", f"rinv{par}"], writes=[f"xtmp{par}"])
                P.dve(lambda e, hd=hd, xt=xt: e.tensor_tensor(out=yT[:, 12 + hd, :], in0=xt[:], in1=yT[:, 12 + hd, :],
                                                              op=ALU.mult),
                      reads=[f"xtmp{par}", f"yT{12 + hd}"], writes=[f"yT{12 + hd}"])

        def wo_prefetch(L):
            if L == 0:
                dead = [[f"Q{c}" for c in range(NCH)] + [f"K{c}" for c in range(NCH)],
                        [f"V{c}" for c in range(NCH)] + ["qT0", "qT1"]]
            else:
                dead = [[f"conv{cb}" for cb in range(8)],
                        [f"conv{cb}" for cb in range(8, 12)] + ["sig0", "sig1", "yb0", "yb1", "ysq0", "ysq1", "MEAN"]]
            for nb in range(2):
                P.dma("sp", f"wo{nb}", lambda e, nb=nb: e.dma_start(out=Wo[nb], in_=wout_s[L][nb]),
                      reads=[f"wout{L}_{nb}"], writes=[f"Wo{nb}"] + dead[nb])

        def out_proj(L, tok0, last_layer):
            fence()
            ykeys = [f"yT{m}" for m in range(16)]
            for c in range(NCH):
                b0 = 4 + 2 * (c % 2)
                for nb in range(2):
                    mm_group(bank(b0 + nb), [(yT[:, m, c * 128:(c + 1) * 128], Wo[nb][:, m, :]) for m in range(16)],
                             ykeys + [f"Wo{nb}"] + ARENA_KEYS, f"ps{b0 + nb}")
                ss2, kss2 = stat(2)
                rs, krs = stat(1)
                for nb in range(2):
                    P.act(lambda e, nb=nb, ss2=ss2, b0=b0: e.activation(out=junk[:, 0:512], in_=bank(b0 + nb), func=AF.Square,
                                                                 accum_out=ss2[:, nb:nb + 1]),
                          reads=[f"ps{b0 + nb}"], writes=["junk", kss2 + f"_{nb}"])
                P.dve(lambda e, ss2=ss2, rs=rs: e.tensor_tensor(out=rs, in0=ss2[:, 0:1], in1=ss2[:, 1:2], op=ALU.add),
                      reads=[kss2 + "_0", kss2 + "_1"], writes=[krs])
                P.dve(lambda e, rs=rs: e.tensor_scalar(out=rs, in0=rs, scalar1=1.0 / D, scalar2=EPS,
                                                      op0=ALU.mult, op1=ALU.add),
                      reads=[krs], writes=[krs])
                P.pool(lambda e, rs=rs: e.tensor_tensor(out=rs, in0=rs, in1=mhalf[:, 0:1], op=ALU.pow),
                       reads=[krs, "mhalf"], writes=[krs])
                P.dve(lambda e, b0=b0: e.tensor_tensor(
                    out=otmp, in0=psum[:, b0:b0 + 2, :].rearrange("p a b -> p (a b)"), in1=gpost[:, L, :], op=ALU.mult),
                    reads=[f"ps{b0}", f"ps{b0 + 1}", "gpost"] + ARENA_KEYS, writes=["otmp"])
                P.dve(lambda e, c=c, rs=rs: e.scalar_tensor_tensor(
                    out=X[:, c, :], in0=otmp, scalar=rs, in1=X[:, c, :], op0=ALU.mult, op1=ALU.add),
                    reads=["otmp", krs, f"X{c}"], writes=[f"X{c}"])
                if last_layer:
                    P.dma("sp", f"xs{c}", lambda e, c=c: e.dma_start(
                        out=out_d[tok0 + c * 128: tok0 + (c + 1) * 128, :], in_=X[:, c, :]),
                        reads=[f"X{c}"])
            fence()

        IPB = [0, 1, 4, 5, 6, 7]

        def feature_major_blocks(nblk, first_mc, silu):
            for b in range(nblk):
                w, wk = w_next()
                for s in range(4):
                    bk = IPB[(b * 4 + s) % len(IPB)]
                    mm_group(bank(bk), [(w[:, k, s * 128:(s + 1) * 128], hT[:, k, :]) for k in range(8)],
                             [wk] + hT_keys, f"ps{bk}")
                    mc = first_mc + 4 * b + s
                    if silu:
                        P.act(lambda e, mc=mc, bk=bk: e.activation(out=yT[:, mc, :], in_=bank(bk), func=AF.Silu),
                              reads=[f"ps{bk}"], writes=[f"yT{mc}"])
                    else:
                        P.act(lambda e, mc=mc, bk=bk: e.activation(out=xqT[:, mc, :], in_=bank(bk), func=AF.Copy),
                              reads=[f"ps{bk}"], writes=["xqT"])
                w_issue()

        def layer0(gi, gs, tok0):
            if DBG_STAGE < 1:
                return
            norm_transpose_group(0)
            if DBG_STAGE < 2:
                return
            cosb = rope[:, 0]
            sinb = rope[:, 1]
            nsinb = rope[:, 2]
            for blk in range(4):
                w, wk = w_next()
                dst = Qb if blk < 2 else Kb
                dkey = "Q" if blk < 2 else "K"
                col0 = (blk % 2) * 512
                for c in range(NCH):
                    bk = IPB[(blk * NCH + c) % len(IPB)]
                    mm_group(bank(bk), [(hT[:, k, c * 128:(c + 1) * 128], w[:, k, :]) for k in range(8)],
                             [wk, f"hT{c}"] + ARENA_KEYS, f"ps{bk}")
                    t1 = t1b[c % 2]
                    t2 = t2b[c % 2]
                    p4 = bank(bk).rearrange("p (h two d) -> p h two d", h=4, two=2)
                    t14 = t1.rearrange("p (h two d) -> p h two d", h=4, two=2)
                    t24 = t2.rearrange("p (h two d) -> p h two d", h=4, two=2)
                    P.dve(lambda e, c=c, p4=p4, t14=t14: e.tensor_tensor(
                        out=t14, in0=p4, in1=cosb[:, c, :].unsqueeze(1).unsqueeze(1).broadcast_to([128, 4, 2, 64]),
                        op=ALU.mult), reads=[f"ps{bk}", "rope"] + ARENA_KEYS, writes=[f"t1_{c % 2}"])

                    def frot(e, c=c, p4=p4, t24=t24):
                        e.tensor_tensor(out=t24[:, :, 0, :], in0=p4[:, :, 1, :],
                                        in1=nsinb[:, c, :].unsqueeze(1).broadcast_to([128, 4, 64]), op=ALU.mult)
                        return e.tensor_tensor(out=t24[:, :, 1, :], in0=p4[:, :, 0, :],
                                               in1=sinb[:, c, :].unsqueeze(1).broadcast_to([128, 4, 64]), op=ALU.mult)
                    P.dve(frot, reads=[f"ps{bk}", "rope"] + ARENA_KEYS, writes=[f"t2_{c % 2}"])
                    P.pool(lambda e, c=c, t1=t1, t2=t2, dst=dst, col0=col0: e.tensor_tensor(
                        out=dst[:, c, col0:col0 + 512], in0=t1, in1=t2, op=ALU.add),
                        reads=[f"t1_{c % 2}", f"t2_{c % 2}"] + ARENA_KEYS, writes=[f"{dkey}{c}"])
                w_issue()
            if DBG_STAGE < 3:
                return
            for vb in range(3):
                w, wk = w_next()
                for c in range(NCH):
                    bk = IPB[(vb * NCH + c) % len(IPB)]
                    mm_group(bank(bk), [(hT[:, k, c * 128:(c + 1) * 128], w[:, k, :]) for k in range(8)],
                             [wk, f"hT{c}"], f"ps{bk}")

                    def fv(e, c=c, bk=bk, vb=vb):
                        ins = None
                        for h in range(NH):
                            lo = max(DV * h, 512 * vb)
                            hi = min(DV * h + DV, 512 * vb + 512)
                            if lo >= hi:
                                continue
                            ins = e.activation(out=Vb[:, c, lo:hi], in_=bank(bk)[:, lo - 512 * vb: hi - 512 * vb],
                                               func=AF.Copy, scale=zs[:, h:h + 1])
                        return ins
                    P.act(fv, reads=[f"ps{bk}", "zs"] + ARENA_KEYS, writes=[f"V{c}"])
                w_issue()
            if DBG_STAGE < 4:
                return
            feature_major_blocks(1, 0, False)
            feature_major_blocks(4, 0, True)
            if DBG_STAGE < 5:
                return
            if gs == 0:
                P.dve(lambda e: e.memset(state[:], 0.0), writes=["state0", "state1"])
                P.pool(lambda e: e.memset(state_bf[:], 0.0), writes=["stbf0", "stbf1"])
            def head(c):
                bi = c % 2
                for (src, skey, dstT, dkey, bk, scaled) in ((Qb, "Q", qT[bi], f"qT{bi}", 0, True),
                                                            (Kb, "K", kT[bi], f"kT{bi}", 1, False)):
                    pb = bank16(bk)

                    def ftr(e, src=src, pb=pb, c=c):
                        ins = None
                        for h in range(NH):
                            ins = e.transpose(out=pb[:, h * 128:(h + 1) * 128], in_=src[:, c, h * 128:(h + 1) * 128],
                                              identity=ident[:])
                        return ins
                    P.pe(ftr, reads=[f"{skey}{c}", "ident"] + ARENA_KEYS, writes=[f"ps{bk}"])
                    if scaled:
                        P.dve(lambda e, pb=pb, dstT=dstT: e.tensor_tensor(
                            out=dstT, in0=pb.rearrange("p (h t) -> p h t", h=NH), in1=xi[:], op=ALU.mult),
                            reads=[f"ps{bk}", "xi"] + ARENA_KEYS, writes=[dkey])
                    else:
                        P.act(lambda e, pb=pb, dstT=dstT: e.activation(
                            out=dstT, in_=pb.rearrange("p (h t) -> p h t", h=NH), func=AF.Copy),
                            reads=[f"ps{bk}"] + ARENA_KEYS, writes=[dkey])
            def halves(c, hh_list, phase="AB"):
                bi = c % 2
                for hh in hh_list:
                    hs = list(range(4 * hh, 4 * hh + 4))
                    ST = STb[2 * bi + hh]
                    skey = f"ST{2 * bi + hh}"
                    ob = 3 if hh == 0 else 5
                    okeys = [f"ps{ob}", f"ps{ob + 1}"]
                    if "A" in phase:
                        def fs(e, hs=hs, bi=bi):
                            ins = None
                            for n, h in enumerate(hs):
                                ins = e.matmul(bank(2)[:, n * 128:(n + 1) * 128], lhsT=kT[bi][:, h, :], rhs=qT[bi][:, h, :],
                                               start=True, stop=True)
                            return ins
                        P.pe(fs, reads=[f"qT{bi}", f"kT{bi}"] + ARENA_KEYS, writes=["ps2"])
                        P.dve(lambda e, ST=ST, hh=hh: e.tensor_tensor(
                            out=ST, in0=bank(2).rearrange("p (h t) -> p h t", h=4), in1=mask[:, 4 * hh:4 * hh + 4, :],
                            op=ALU.mult), reads=["ps2", "mask"] + ARENA_KEYS, writes=[skey])
                        if DBG_STAGE < 4.2:
                            continue

                        def fo(e, hs=hs, bi=bi, ST=ST, c=c, ob=ob):
                            ins = None
                            for n, h in enumerate(hs):
                                o = bank(ob + n // 2)[:, (n % 2) * DV:(n % 2) * DV + DV]
                                e.matmul(o, lhsT=ST[:, n, :], rhs=Vb[:, c, h * DV:(h + 1) * DV], start=True, stop=False)
                                ins = e.matmul(o, lhsT=qT[bi][:, h, :], rhs=state_bf[:, h, :], start=False, stop=True)
                            return ins
                        P.pe(fo, reads=[skey, f"V{c}", f"qT{bi}", f"stbf{hh}"] + ARENA_KEYS, writes=okeys)
                        if DBG_STAGE < 4.3:
                            continue
                        last_chunk = False
                        if not last_chunk:
                            for pp in range(2):
                                def fd(e, hs=hs, c=c, pp=pp):
                                    ins = None
                                    for n in (2 * pp, 2 * pp + 1):
                                        h = hs[n]
                                        o = bank(7)[:, (n % 2) * DV:(n % 2) * DV + DV]
                                        ins = e.matmul(o, lhsT=Kb[:, c, h * 128:(h + 1) * 128],
                                                       rhs=Vb[:, c, h * DV:(h + 1) * DV], start=True, stop=True)
                                    return ins
                                P.pe(fd, reads=[f"K{c}", f"V{c}"] + ARENA_KEYS, writes=["ps7"])

                                def fst(e, hs=hs, pp=pp):
                                    ins = None
                                    for n in (2 * pp, 2 * pp + 1):
                                        h = hs[n]
                                        o = bank(7)[:, (n % 2) * DV:(n % 2) * DV + DV]
                                        ins = e.scalar_tensor_tensor(out=state[:, h, :], in0=state[:, h, :], scalar=cdec[h],
                                                                     in1=o, op0=ALU.mult, op1=ALU.add)
                                    return ins
                                P.dve(fst, reads=["ps7", f"state{hh}"], writes=[f"state{hh}"])
                            P.act(lambda e, hh=hh: e.activation(out=state_bf[:, 4 * hh:4 * hh + 4, :],
                                                               in_=state[:, 4 * hh:4 * hh + 4, :], func=AF.Copy),
                                  reads=[f"state{hh}"], writes=[f"stbf{hh}"])
                    if "B" in phase:
                        if DBG_STAGE < 4.4 or "stats" in SKIP:
                            continue
                        o4 = psum[:, ob:ob + 2, 0:2 * DV].rearrange("p b (h v) -> p b h v", h=2)
                        sm, ksm = stat(4)
                        sq, ksq = stat(4)
                        mu, kmu = stat(4)
                        rs, krs = stat(4)
                        nm, knm = stat(4)
                        sqs = t1b[0].rearrange("p (b h v) -> p b h v", b=2, h=2)[:, :, :, 0:DV] if False else None
                        P.dve(lambda e, sm=sm, o4=o4: e.tensor_reduce(
                            out=sm.rearrange("p (b h) -> p b h", b=2), in_=o4, axis=AX.X, op=ALU.add),
                            reads=okeys, writes=[ksm])

                        def fsq(e, sq=sq, ob=ob):
                            ins = None
                            for n in range(4):
                                o = bank(ob + n // 2)[:, (n % 2) * DV:(n % 2) * DV + DV]
                                ins = e.activation(out=junk[:, n * DV:(n + 1) * DV], in_=o, func=AF.Square, accum_out=sq[:, n:n + 1])
                            return ins
                        P.act(fsq, reads=okeys, writes=["junk", ksq])
                        P.dve(lambda e, sm=sm, mu=mu: e.tensor_scalar(out=mu, in0=sm, scalar1=1.0 / DV, scalar2=None,
                                                                     op0=ALU.mult), reads=[ksm], writes=[kmu])
                        P.dve(lambda e, mu=mu, nm=nm: e.tensor_tensor(out=nm, in0=mu, in1=mu, op=ALU.mult),
                              reads=[kmu], writes=[knm])
                        P.dve(lambda e, sq=sq, nm=nm, rs=rs: e.scalar_tensor_tensor(
                            out=rs, in0=sq, scalar=1.0 / DV, in1=nm, op0=ALU.mult, op1=ALU.subtract),
                            reads=[ksq, knm], writes=[krs])
                        P.dve(lambda e, rs=rs: e.tensor_scalar(out=rs, in0=rs, scalar1=EPS, scalar2=None, op0=ALU.add),
                              reads=[krs], writes=[krs])
                        P.pool(lambda e, rs=rs: e.tensor_tensor(out=rs, in0=rs, in1=mhalf[:, 0:4],
                                                                op=ALU.pow), reads=[krs, "mhalf"], writes=[krs])
                        P.dve(lambda e, mu=mu, rs=rs, nm=nm: e.scalar_tensor_tensor(
                            out=nm, in0=mu, scalar=-1.0, in1=rs, op0=ALU.mult, op1=ALU.mult),
                            reads=[kmu, krs], writes=[knm])
                        if DBG_STAGE < 4.5 or "fap" in SKIP:
                            continue
                        ON = ONb[bi]

                        def fap(e, hs=hs, rs=rs, nm=nm, ON=ON, ob=ob):
                            ins = None
                            for n, h in enumerate(hs):
                                o = bank(ob + n // 2)[:, (n % 2) * DV:(n % 2) * DV + DV]
                                ins = e.activation(out=ON[:, h * DV:(h + 1) * DV], in_=o, func=AF.Identity,
                                                   scale=rs[:, n:n + 1], bias=nm[:, n:n + 1])
                            return ins
                        P.act(fap, reads=okeys + [krs, knm] + ARENA_KEYS, writes=[f"ON{bi}_{hh}"])

            def tail(c):
                bi = c % 2
                ON = ONb[bi]
                for p_ in range(2):
                    m0, m1 = 6 * p_, 6 * p_ + 6

                    def ftr2(e, ON=ON, m0=m0, m1=m1):
                        ins = None
                        for m in range(m0, m1):
                            ins = e.transpose(out=bank16(2)[:, (m - m0) * 128:(m - m0 + 1) * 128],
                                              in_=ON[:, m * 128:(m + 1) * 128], identity=ident[:])
                        return ins
                    P.pe(ftr2, reads=[f"ON{bi}_0", f"ON{bi}_1", "ident"], writes=["ps2"])
                    tmpb = ontT[:, m0:m1, :]
                    P.act(lambda e, tmpb=tmpb: e.activation(
                        out=tmpb, in_=bank16(2)[:, 0:6 * 128].rearrange("p (m t) -> p m t", m=6), func=AF.Copy),
                        reads=["ps2"], writes=[f"ontT{p_}"])
                    P.dve(lambda e, c=c, m0=m0, m1=m1, tmpb=tmpb: e.tensor_tensor(
                        out=yT[:, m0:m1, c * 128:(c + 1) * 128], in0=tmpb,
                        in1=yT[:, m0:m1, c * 128:(c + 1) * 128], op=ALU.mult),
                        reads=[f"ontT{p_}"] + [f"yT{m}" for m in range(m0, m1)],
                        writes=[f"yT{m}" for m in range(m0, m1)])

            head(0)
            halves(0, [0, 1], "A")
            halves(0, [0, 1], "B")
            for c in range(1, NCH):
                head(c)
                halves(c, [0, 1], "A")
                tail(c - 1)
                halves(c, [0, 1], "B")
            tail(NCH - 1)
            if "pad" in SKIP:
                for i in range(60):
                    if "padscale" in SKIP:
                        P.act(lambda e: e.activation(out=xtmp[:, 0:16], in_=rinv[:, 0:16], func=AF.Copy, scale=zs[:, 0:1]),
                              reads=["rinv", "zs"], writes=["xtmp"])
                    elif "padbias" in SKIP:
                        P.act(lambda e: e.activation(out=xtmp[:, 0:16], in_=rinv[:, 0:16], func=AF.Identity, bias=zs[:, 0:1]),
                              reads=["rinv", "zs"], writes=["xtmp"])
                    else:
                        P.act(lambda e: e.activation(out=xtmp[:, 0:16], in_=rinv[:, 0:16], func=AF.Copy),
                              reads=["rinv"], writes=["xtmp"])
            if DBG_STAGE < 6:
                return
            wo_prefetch(0)
            mem_attn()
            if DBG_STAGE < 7:
                return
            out_proj(0, tok0, last_layer=(1 not in layers))

        def layer1(gi, gs, tok0):
            norm_transpose_group(1)
            if gs == 0:
                P.pool(lambda e: e.memset(gluT[:, :, 0:30], 0.0), writes=["gluhist"])
                P.pool(lambda e: e.memset(gluT[:, :, T + 30:T + 32], 0.0), writes=["glupad"])
            for i in range(3):
                wu, ku = w_next()
                wg, kg = w_next()
                for s in range(4):
                    cb = 4 * i + s
                    ba = 2 * (s % 2)
                    mm_group(bank(ba), [(wu[:, k, s * 128:(s + 1) * 128], hT[:, k, :]) for k in range(8)],
                             [ku] + hT_keys, f"ps{ba}")
                    mm_group(bank(ba + 1), [(wg[:, k, s * 128:(s + 1) * 128], hT[:, k, :]) for k in range(8)],
                             [kg] + hT_keys, f"ps{ba + 1}")
                    sg = sigb[s % 2]
                    P.act(lambda e, sg=sg, ba=ba: e.activation(out=sg, in_=bank(ba + 1), func=AF.Sigmoid),
                          reads=[f"ps{ba + 1}"] + ARENA_KEYS, writes=[f"sig{s % 2}"])
                    P.dve(lambda e, sg=sg, ba=ba, cb=cb: e.tensor_tensor(
                        out=gluT[:, cb, 30:30 + T], in0=bank(ba), in1=sg, op=ALU.mult),
                        reads=[f"ps{ba}", f"sig{s % 2}"] + ARENA_KEYS, writes=[f"glu{cb}"])
                w_issue()
                w_issue()
            feature_major_blocks(1, 0, False)
            feature_major_blocks(4, 0, True)
            def build_b(cb):
                dB = diagB2[cb % 2]

                def fdb(e, cb=cb, dB=dB):
                    ins = None
                    for j in range(15):
                        ins = e.activation(out=dB[:, j, :], in_=ident[:], func=AF.Copy, scale=dw[:, cb, 16 + j:17 + j])
                    return ins
                P.act(fdb, reads=["ident", "dw"], writes=[f"diagB{cb % 2}"])

            build_b(0)
            for cb in range(12):
                bk = 6 + (cb % 2)
                dB = diagB2[cb % 2]

                def fda(e, cb=cb):
                    ins = None
                    for j in range(16):
                        ins = e.tensor_scalar(out=diagA[:, j, :], in0=identf[:], scalar1=dw[:, cb, j:j + 1], scalar2=None,
                                              op0=ALU.mult)
                    return ins
                P.dve(fda, reads=["identf", "dw"], writes=["diagA"])
                if cb + 1 < 12:
                    build_b(cb + 1)

                def fca(e, cb=cb, bk=bk):
                    ins = None
                    for j in range(16):
                        ins = e.matmul(bank(bk), lhsT=diagA[:, j, :], rhs=gluT[:, cb, j:j + T], start=(j == 0), stop=False)
                    return ins
                P.pe(fca, reads=["diagA", f"glu{cb}", "gluhist"], writes=[f"ps{bk}"])

                def fcb(e, cb=cb, bk=bk, dB=dB):
                    ins = None
                    for j in range(15):
                        k = 16 + j
                        ins = e.matmul(bank(bk), lhsT=dB[:, j, :], rhs=gluT[:, cb, k:k + T], start=False, stop=(j == 14))
                    return ins
                P.pe(fcb, reads=[f"diagB{cb % 2}", f"glu{cb}", "gluhist"], writes=[f"ps{bk}"])
                P.act(lambda e, cb=cb, bk=bk: e.activation(out=convT[:, cb, :], in_=bank(bk), func=AF.Identity,
                                                           bias=dwb[:, cb:cb + 1]),
                      reads=[f"ps{bk}", "dwb"], writes=[f"conv{cb}"])
            P.act(lambda e: e.activation(out=gluT[:, :, 0:30], in_=gluT[:, :, T:T + 30], func=AF.Copy),
                  reads=[f"glu{cb}" for cb in range(12)] + ["gluhist"], writes=["gluhist"])
            for cb in range(12):
                yb = ybb[cb % 2]
                ysq = ysqb[cb % 2]
                P.act(lambda e, cb=cb, yb=yb: e.activation(out=yb, in_=convT[:, cb, :], func=AF.Copy),
                      reads=[f"conv{cb}"], writes=[f"yb{cb % 2}"])
                P.act(lambda e, cb=cb, ysq=ysq: e.activation(out=ysq, in_=convT[:, cb, :], func=AF.Square),
                      reads=[f"conv{cb}"], writes=[f"ysq{cb % 2}"])
                P.pe(lambda e, cb=cb, yb=yb: e.matmul(bank(4), lhsT=ones[:], rhs=yb, start=(cb == 0), stop=(cb == 11)),
                     reads=["ones", f"yb{cb % 2}"], writes=["ps4"])
                P.pe(lambda e, cb=cb, ysq=ysq: e.matmul(bank(5), lhsT=ones[:], rhs=ysq, start=(cb == 0),
                                                        stop=(cb == 11)),
                     reads=["ones", f"ysq{cb % 2}"], writes=["ps5"])
            P.act(lambda e: e.activation(out=MEAN, in_=bank(4), func=AF.Copy, scale=1.0 / BW),
                  reads=["ps4"], writes=["MEAN"])
            P.dve(lambda e: e.tensor_tensor(out=VAR, in0=MEAN, in1=MEAN, op=ALU.mult), reads=["MEAN"], writes=["VAR"])
            P.dve(lambda e: e.scalar_tensor_tensor(out=RSTD, in0=bank(5), scalar=1.0 / BW, in1=VAR,
                                                   op0=ALU.mult, op1=ALU.subtract),
                  reads=["ps5", "VAR"], writes=["RSTD"])
            P.dve(lambda e: e.tensor_scalar(out=VAR, in0=RSTD, scalar1=EPS, scalar2=None, op0=ALU.add),
                  reads=["RSTD"], writes=["VAR"])
            P.dve(lambda e: e.reciprocal(out=VAR, in_=VAR), reads=["VAR"], writes=["VAR"])
            P.act(lambda e: e.activation(out=RSTD, in_=VAR, func=AF.Sqrt), reads=["VAR"], writes=["RSTD"])
            P.dve(lambda e: e.scalar_tensor_tensor(out=NMR, in0=MEAN, scalar=-1.0, in1=RSTD, op0=ALU.mult, op1=ALU.mult),
                  reads=["MEAN", "RSTD"], writes=["NMR"])
            for cb in range(12):
                z = zb[cb % 2]
                a = ab[cb % 2]
                P.dve(lambda e, cb=cb, z=z: e.tensor_tensor(out=z, in0=convT[:, cb, :], in1=RSTD, op=ALU.mult),
                      reads=[f"conv{cb}", "RSTD"], writes=[f"z{cb % 2}"])
                P.dve(lambda e, z=z: e.tensor_tensor(out=z, in0=z, in1=NMR, op=ALU.add),
                      reads=[f"z{cb % 2}", "NMR"], writes=[f"z{cb % 2}"])
                P.act(lambda e, cb=cb, z=z, a=a: e.activation(out=a, in_=z, func=AF.Silu, scale=lng[:, cb:cb + 1],
                                                             bias=lnb[:, cb:cb + 1]),
                      reads=[f"z{cb % 2}", "lng", "lnb"], writes=[f"a{cb % 2}"])
                P.dve(lambda e, cb=cb, a=a: e.tensor_tensor(out=yT[:, cb, :], in0=a, in1=yT[:, cb, :], op=ALU.mult),
                      reads=[f"a{cb % 2}", f"yT{cb}"], writes=[f"yT{cb}"])
                if cb % 3 == 1:
                    mem_attn((cb // 3,))
            wo_prefetch(1)
            out_proj(1, tok0, last_layer=True)

        for s in range(nseq):
            mem_kv(s)
            for gs in range(ngs):
                gi = s * ngs + gs
                tok0 = s * seqlen + gs * T
                for c in range(NCH):
                    P.dma("sp", f"xl{c}", lambda e, c=c, tok0=tok0: e.dma_start(
                        out=X[:, c, :], in_=x_d[tok0 + c * 128: tok0 + (c + 1) * 128, :]), writes=[f"X{c}"])
                P.dma("sp", "ropeld", lambda e, gi=gi: e.dma_start(
                    out=rope[:], in_=rope_s[:, :, gi * NCH:(gi + 1) * NCH, :].rearrange("w p c d -> p w c d")),
                    reads=[f"rope_s{gi}"], writes=["rope", "rope0", "rope1", "rope2"])
                if 0 in layers:
                    layer0(gi, gs, tok0)
                if 1 in layers:
                    layer1(gi, gs, tok0)
        with nc.allow_low_precision("bf16 matmul operands, fp32 accumulation"):
            P.finish()
        build_program.last_prog = P
    return nc


def _core_inputs(i, nseq, inputs, consts):
    f = np.float32
    sl = slice(i * nseq, (i + 1) * nseq)
    x = np.ascontiguousarray(inputs["x"][sl]).reshape(-1, D)
    mem = np.ascontiguousarray(inputs["mem"][sl]).reshape(-1, D)
    pos = np.ascontiguousarray(inputs["positions"][sl]).reshape(-1)
    ncht = pos.shape[0] // 128
    m = {
        "x": x, "mem": mem,
        "pos": np.ascontiguousarray(pos.reshape(ncht, 128).T).astype(np.int32),
        "w_mem_kv": inputs["w_mem_kv"],
        "ret_w_in": inputs["ret_w_in"][0], "conv_w_in": inputs["conv_w_in"][0],
        "ret_w_out": inputs["ret_w_out"][0], "conv_w_out": inputs["conv_w_out"][0],
        "g_mem": np.ascontiguousarray(inputs["mem_norm_g"].reshape(8, 128).T),
        "g_pre": np.ascontiguousarray(inputs["norm_pre_g"].reshape(2, 8, 128).transpose(2, 0, 1)),
        "g_post": np.ascontiguousarray(np.broadcast_to(inputs["norm_post_g"][None], (128, 2, D))),
        "dw_w": np.ascontiguousarray(inputs["conv_dw_w"][0].reshape(CW, 12, 128).transpose(2, 1, 0)),
        "dw_b": np.ascontiguousarray(inputs["conv_dw_b"][0].reshape(12, 128).T),
        "ln_g": np.ascontiguousarray(inputs["conv_ln_g"][0].reshape(12, 128).T),
        "ln_b": np.ascontiguousarray(inputs["conv_ln_b"][0].reshape(12, 128).T),
    }
    m.update(consts)
    return {k: np.ascontiguousarray(v) for k, v in m.items()}


_CACHE = {}


def run(inputs, n_cores, nseq, seqlen, layers=(0, 1), trace=False):
    key = (nseq, seqlen, tuple(layers))
    if key not in _CACHE:
        _CACHE[key] = build_program(nseq, seqlen, layers)
    nc = _CACHE[key]
    consts, _ = make_consts()
    inputs = {k: np.asarray(v) for k, v in inputs.items()}
    in_maps = [_core_inputs(i, nseq, inputs, consts) for i in range(n_cores)]
    res = run_bass_kernel_spmd(nc, in_maps, core_ids=list(range(n_cores)), **({"trace": True} if trace else {}))
    out = np.stack([r["out"].reshape(nseq, seqlen, D) for r in res.results], axis=0)
    return out.reshape(n_cores * nseq, seqlen, D), res


def kernel(x, mem, positions, mem_norm_g, w_mem_kv, norm_pre_g, norm_post_g, ret_w_in, ret_w_out,
           conv_w_in, conv_dw_w, conv_dw_b, conv_ln_g, conv_ln_b, conv_w_out):
    inputs = dict(x=x, mem=mem, positions=positions, mem_norm_g=mem_norm_g, w_mem_kv=w_mem_kv,
                  norm_pre_g=norm_pre_g, norm_post_g=norm_post_g, ret_w_in=ret_w_in, ret_w_out=ret_w_out,
                  conv_w_in=conv_w_in, conv_dw_w=conv_dw_w, conv_dw_b=conv_dw_b, conv_ln_g=conv_ln_g,
                  conv_ln_b=conv_ln_b, conv_w_out=conv_w_out)
    out, _ = run(inputs, 8, 2, 2048)
    return out.astype(np.float32)
```

```python
import math
import re
import numpy as np
from contextlib import ExitStack
import concourse.bass as bass
import concourse.mybir as mybir
from concourse.bass_utils import run_bass_kernel_spmd

F32 = mybir.dt.float32
BF16 = mybir.dt.bfloat16
I32 = mybir.dt.int32
AF = mybir.ActivationFunctionType
ALU = mybir.AluOpType
AX = mybir.AxisListType

D = 1024
MEM = 256
EPS = 1e-6
NH = 8
DV = 192
BW = 1536
CW = 31
NCH = 4
T = NCH * 128
RET_IN = 6144
CONV_IN = 5632
MAGIC = 12582912.0
DBG_STAGE = 99
SKIP = set()
TWO_PI = 2.0 * math.pi


PS_RE = re.compile(r"^ps\d$")
ARENA_RE = re.compile(r"^(diag|Q\d|K\d|V\d|VAR|qT|kT|ST\d|ON|t1_|t2_|Wo|otmp|conv|sig|yb|ysq|MEAN|RSTD|NMR|z\d|a\d)")


class Prog:
    ENGS = ("pe", "dve", "act", "pool", "sp")

    def __init__(self, nc, stack):
        self.nc = nc
        self.stack = stack
        self.ops = []
        self.last_w = {}
        self.readers = {}

    def sb(self, name, shape, dtype):
        return self.stack.enter_context(self.nc.sbuf_tensor(name, list(shape), dtype))

    def ps(self, name, shape, dtype):
        return self.stack.enter_context(self.nc.psum_tensor(name, list(shape), dtype))

    def op(self, eng, fn, reads=(), writes=(), dma=None, ndma=1):
        idx = len(self.ops)
        deps = set()
        is_dma = dma is not None
        reads = list(reads)
        writes = list(writes)
        if "ar_all" not in writes and any(ARENA_RE.match(k) for k in reads + writes):
            reads.append("ar_all")
        for k in reads:
            if PS_RE.match(k) and k not in writes:
                writes.append(k)

        def add(d, raw):
            if d is None or d == idx:
                return
            od = self.ops[d]
            deps.add(d)
        for r in reads:
            add(self.last_w.get(r), True)
        for w in writes:
            add(self.last_w.get(w), False)
            for rd in self.readers.get(w, ()):
                add(rd, False)
        for r in reads:
            self.readers.setdefault(r, []).append(idx)
        for w in writes:
            self.last_w[w] = idx
            self.readers[w] = []
        import sys as _sys
        fr = _sys._getframe(1)
        while fr.f_code.co_name in ('op', 'pe', 'dve', 'act', 'pool', 'dma', 'mm_group'):
            fr = fr.f_back
        self.ops.append(dict(eng=eng, fn=fn, deps=deps, dma=dma, ndma=ndma, line=fr.f_lineno))
        return idx

    def pe(self, fn, reads=(), writes=()):
        return self.op("pe", fn, reads, writes)

    def dve(self, fn, reads=(), writes=()):
        return self.op("dve", fn, reads, writes)

    def act(self, fn, reads=(), writes=()):
        return self.op("act", fn, reads, writes)

    def pool(self, fn, reads=(), writes=()):
        return self.op("pool", fn, reads, writes)

    def dma(self, queue, semname, fn, reads=(), writes=(), ndma=1):
        return self.op(queue, fn, reads, writes, dma=semname, ndma=ndma)

    def finish(self, final_wait_eng="sp"):
        nc = self.nc
        ops = self.ops
        n = len(ops)
        needed = set()
        for o in ops:
            needed |= o["deps"]
        sem_names = set(self.ENGS)
        for o in ops:
            if o["dma"] is not None:
                sem_names.add("dma:" + o["dma"])
        sems = {}
        for s in sorted(sem_names):
            sems[s] = self.stack.enter_context(nc.semaphore(s.replace(":", "_")))
        counts = {s: 0 for s in sem_names}
        ev = [None] * n
        vc = [None] * n
        clock = {e: {} for e in self.ENGS}
        streams = {e: [] for e in self.ENGS}
        outstanding = {}
        for i, o in enumerate(ops):
            e = o["eng"]
            ck = clock[e]
            waits = {}
            for d in sorted(o["deps"]):
                s, v = ev[d]
                if ck.get(s, 0) >= v:
                    continue
                if waits.get(s, 0) < v:
                    waits[s] = v
                for s2, v2 in vc[d].items():
                    if ck.get(s2, 0) < v2:
                        ck[s2] = v2
            if o["dma"] is not None:
                s = "dma:" + o["dma"]
                counts[s] += 16 * o["ndma"]
                ev[i] = (s, counts[s])
                inc = (s, 16)
                outstanding[s] = counts[s]
                v = dict(ck)
                v[s] = counts[s]
                vc[i] = v
            elif i in needed:
                counts[e] += 1
                ev[i] = (e, counts[e])
                inc = (e, 1)
                v = dict(ck)
                v[e] = counts[e]
                vc[i] = v
            else:
                inc = None
            streams[e].append((waits, o["fn"], inc))
            o["waits"] = dict(waits)
            o["ev"] = ev[i]
        self.sem_counts = counts
        engobj = {"pe": "tensor", "dve": "vector", "act": "scalar", "pool": "gpsimd", "sp": "sync"}
        with nc.Block() as block:
            for e in self.ENGS:
                stream = streams[e]
                fin = outstanding if e == final_wait_eng else None

                def body(eng, stream=stream, fin=fin):
                    for waits, fn, inc in stream:
                        for s, v in waits.items():
                            eng.wait_ge(sems[s], v)
                        ins = fn(eng)
                        if inc is not None:
                            if isinstance(ins, (list, tuple)):
                                for x in ins:
                                    x.then_inc(sems[inc[0]], inc[1])
                            else:
                                ins.then_inc(sems[inc[0]], inc[1])
                    if fin:
                        for s, v in fin.items():
                            eng.wait_ge(sems[s], v)
                getattr(block, engobj[e])(body)


def _gammas():
    h = np.arange(NH, dtype=np.float64)
    return 1.0 - np.exp2(-5.0 - h)


def make_consts():
    g = _gammas()
    idx = np.arange(128, dtype=np.float64)
    c = {}
    c["c_ident"] = np.eye(128, dtype=np.float32)
    c["c_ones"] = np.ones((128, 128), dtype=np.float32)
    half = 64
    inv = (10000.0 ** (-(np.arange(half, dtype=np.float32) / np.float32(half)))).astype(np.float32)
    c["c_invf"] = np.ascontiguousarray(np.broadcast_to(inv[None, :], (128, half))).astype(np.float32)
    causal = (idx[None, :] >= idx[:, None]).astype(np.float64)
    mask = causal[:, None, :] * (g ** -128.0)[None, :, None]
    c["c_mask"] = mask.astype(np.float32)
    xi = g[:, None] ** (idx[None, :] + 1.0)
    c["c_xi"] = np.ascontiguousarray(np.broadcast_to(xi[None], (128, NH, 128))).astype(np.float32)
    zs = (g[None, :] ** (127.0 - idx[:, None])) * (128.0 ** -0.5)
    c["c_zs"] = zs.astype(np.float32)
    return c, [float(x) for x in (g ** 128.0)]


def build_program(nseq, seqlen, layers=(0, 1)):
    ngs = seqlen // T
    ng = nseq * ngs
    ntok = nseq * seqlen
    ncht = ntok // 128
    consts, cdec = make_consts()
    nc = bass.Bass("TRN2", target_bir_lowering=False)

    def din(name, shape, dt=F32):
        return nc.dram_tensor(name, list(shape), dt, kind="ExternalInput").ap()
    x_d = din("x", [ntok, D])
    mem_d = din("mem", [nseq * MEM, D])
    pos_d = din("pos", [128, ncht], I32)
    wkv_d = din("w_mem_kv", [D, D])
    w_in_d = [din("ret_w_in", [D, RET_IN]), din("conv_w_in", [D, CONV_IN])]
    w_out_d = [din("ret_w_out", [2048, D]), din("conv_w_out", [2048, D])]
    gmem_d = din("g_mem", [128, 8])
    gpre_d = din("g_pre", [128, 2, 8])
    gpost_d = din("g_post", [128, 2, D])
    dw_d = din("dw_w", [128, 12, CW])
    dwb_d = din("dw_b", [128, 12])
    lng_d = din("ln_g", [128, 12])
    lnb_d = din("ln_b", [128, 12])
    cd = {k: din(k, v.shape) for k, v in consts.items()}
    out_d = nc.dram_tensor("out", [ntok, D], F32, kind="ExternalOutput").ap()

    wkv_s = nc.dram_tensor("wkv_s", [2, 128, 8, 512], BF16).ap()
    win_s = [nc.dram_tensor("win0_s", [12, 128, 8, 512], BF16).ap(),
             nc.dram_tensor("win1_s", [11, 128, 8, 512], BF16).ap()]
    wout_s = [nc.dram_tensor("wout0_s", [2, 128, 16, 512], BF16).ap(),
              nc.dram_tensor("wout1_s", [2, 128, 16, 512], BF16).ap()]
    rope_s = nc.dram_tensor("rope_s", [3, 128, ncht, 64], F32).ap()

    with ExitStack() as st:
        P = Prog(nc, st)
        X = P.sb("X", [128, NCH, D], F32)
        hb = [P.sb(f"hb{i}", [128, D], BF16) for i in range(2)]
        junk = P.sb("junk", [128, D], BF16)
        hT = P.sb("hT", [128, 8, T], BF16)
        WS = [P.sb(f"WS{i}", [128, 8, 512], BF16) for i in range(3)]
        ARENA_B = 56 * 1024
        arena = P.sb("arena", [128, ARENA_B // 2], BF16)
        arena32 = arena.bitcast(F32)

        def a16(off_bytes, shape):
            n = int(np.prod(shape))
            ap = arena[:, off_bytes // 2: off_bytes // 2 + n]
            return ap

        def a32(off_bytes, shape):
            n = int(np.prod(shape))
            return arena32[:, off_bytes // 4: off_bytes // 4 + n]
        KB = 1024
        Qb = a16(0, [NCH * D]).rearrange("p (c f) -> p c f", c=NCH)
        Kb = a16(8 * KB, [NCH * D]).rearrange("p (c f) -> p c f", c=NCH)
        Vb = a16(16 * KB, [NCH * BW]).rearrange("p (c f) -> p c f", c=NCH)
        qT = [a16(28 * KB + i * 2 * KB, [NH * 128]).rearrange("p (h t) -> p h t", h=NH) for i in range(2)]
        kT = [a16(32 * KB + i * 2 * KB, [NH * 128]).rearrange("p (h t) -> p h t", h=NH) for i in range(2)]
        STb = [a16(36 * KB + i * 1 * KB, [4 * 128]).rearrange("p (h t) -> p h t", h=4) for i in range(4)]
        ONb = [a16(40 * KB + i * 3 * KB, [BW]) for i in range(2)]
        t1b = [a32(46 * KB + i * 2 * KB, [512]) for i in range(2)]
        t2b = [a32(50 * KB + i * 2 * KB, [512]) for i in range(2)]
        Wo = [a16(i * 16 * KB, [16 * 512]).rearrange("p (k n) -> p k n", k=16) for i in range(2)]
        otmp = a32(32 * KB, [D])
        convT = a32(0, [12 * T]).rearrange("p (c t) -> p c t", c=12)
        sigb = [a16(24 * KB + i * KB, [T]) for i in range(2)]
        ybb = [a16(26 * KB + i * KB, [T]) for i in range(2)]
        ysqb = [a16(28 * KB + i * KB, [T]) for i in range(2)]
        MEAN = a32(30 * KB, [T])
        RSTD = a32(32 * KB, [T])
        NMR = a32(34 * KB, [T])
        VAR = a32(36 * KB, [T])
        zb = [a32(38 * KB + i * 2 * KB, [T]) for i in range(2)]
        ab = [a16(42 * KB + i * KB, [T]) for i in range(2)]
        diagA = a16(44 * KB, [16 * 128]).rearrange("p (j c) -> p j c", j=16)
        diagB2 = [a16((48 + 4 * i) * KB, [16 * 128]).rearrange("p (j c) -> p j c", j=16) for i in range(2)]
        ARENA_KEYS = []

        xqT = P.sb("xqT", [128, 4, T], BF16)
        yT = P.sb("yT", [128, 16, T], BF16)
        pT = [P.sb(f"pT{i}", [128, T], BF16) for i in range(4)]
        rinv = P.sb("rinv", [128, T], F32)
        ontT = P.sb("ontT", [128, 12, 128], BF16)
        xtmp = P.sb("xtmp", [128, T], F32)
        rinv2 = [rinv, P.sb("rinv_b", [128, T], F32)]
        xtmp2 = [xtmp, P.sb("xtmp_b", [128, T], F32)]
        state = P.sb("state", [128, NH, DV], F32)
        state_bf = P.sb("state_bf", [128, NH, DV], BF16)
        gluT = P.sb("gluT", [128, 12, T + 32], BF16)
        ident = P.sb("ident", [128, 128], BF16)
        ones = P.sb("ones", [128, 128], BF16)
        identf = P.sb("identf", [128, 128], F32)
        invf = P.sb("invf", [128, 64], F32)
        mask = P.sb("mask", [128, NH, 128], F32)
        xi = P.sb("xi", [128, NH, 128], F32)
        zs = P.sb("zs", [128, NH], F32)
        gmem = P.sb("gmem", [128, 8], F32)
        gpre = P.sb("gpre", [128, 2, 8], F32)
        gpost = P.sb("gpost", [128, 2, D], F32)
        dw = P.sb("dw", [128, 12, CW], F32)
        dwb = P.sb("dwb", [128, 12], F32)
        lng = P.sb("lng", [128, 12], F32)
        lnb = P.sb("lnb", [128, 12], F32)
        mhalf = P.sb("mhalf", [128, T], F32)
        rope = P.sb("rope", [128, 3, NCH, 64], F32)
        posi = P.sb("posi", [128, ncht], I32)
        posf = P.sb("posf", [128, ncht], F32)
        rtmp = [P.sb(f"rtmp{i}", [128, NCH, 64], F32) for i in range(3)]
        memkT = P.sb("memkT", [128, 4, MEM], BF16)
        memv = P.sb("memv", [128, 2, 512], BF16)
        small = P.sb("small", [128, 512], F32)
        fdummy = P.sb("fdummy", [128, 1], F32)
        psum = P.ps("psum", [128, 8, 512], F32)
        psum16 = psum.bitcast(BF16)

        def bank(i):
            return psum[:, i, :]

        def bank16(i):
            return psum16[:, i, :]

        cnt = {"stat": 0}

        def stat(n):
            o = cnt["stat"]
            cnt["stat"] = (o + 1) % 128
            return small[:, 4 * o:4 * o + n], f"small{o}"

        def ld(q, sem, dst, src, key):
            P.dma(q, sem, lambda e: e.dma_start(out=dst, in_=src), writes=[key])
        ld("pool", "c0", ident[:], cd["c_ident"], "ident")
        ld("pool", "c1", ones[:], cd["c_ones"], "ones")
        ld("sp", "c1f", identf[:], cd["c_ident"], "identf")
        ld("sp", "c2", invf[:], cd["c_invf"], "invf")
        ld("sp", "c3", mask[:], cd["c_mask"], "mask")
        ld("sp", "c4", xi[:], cd["c_xi"], "xi")
        ld("sp", "c5", zs[:], cd["c_zs"], "zs")
        ld("sp", "c6", gmem[:], gmem_d, "gmem")
        ld("sp", "c7", gpre[:], gpre_d, "gpre")
        ld("sp", "c8", gpost[:], gpost_d, "gpost")
        ld("sp", "c9", dw[:], dw_d, "dw")
        ld("sp", "c10", dwb[:], dwb_d, "dwb")
        ld("sp", "c11", lng[:], lng_d, "lng")
        ld("sp", "c12", lnb[:], lnb_d, "lnb")
        ld("sp", "c13", posi[:], pos_d, "posi")
        P.pool(lambda e: e.memset(mhalf[:], -0.5), writes=["mhalf"])

        def cast_in(sem, dst, src_w, c0, key):
            P.dma("pool", sem, lambda e: e.dma_start(
                out=dst, in_=src_w.rearrange("(k p) n -> p k n", p=128)[:, :, c0:c0 + 512]), writes=[key])

        def cast_out(sem, dst, src_w, c0, key):
            P.dma("pool", sem, lambda e: e.dma_start(
                out=dst, in_=src_w.rearrange("(k p) n -> p k n", p=128)[:, :, c0:c0 + 512]), writes=[key])
        for b in range(2):
            cast_in(f"ck{b}", wkv_s[b], wkv_d, b * 512, f"wkv_s{b}")
        L0_ORDER = list(range(12))
        L1_ORDER = [0, 3, 1, 4, 2, 5, 6, 7, 8, 9, 10]
        if 0 in layers:
            for b in L0_ORDER:
                cast_in(f"cw0_{b}", win_s[0][b], w_in_d[0], b * 512, f"win0_{b}")
            for b in range(2):
                cast_out(f"co0_{b}", wout_s[0][b], w_out_d[0], b * 512, f"wout0_{b}")
        if 1 in layers:
            for b in L1_ORDER:
                cast_in(f"cw1_{b}", win_s[1][b], w_in_d[1], b * 512, f"win1_{b}")
            for b in range(2):
                cast_out(f"co1_{b}", wout_s[1][b], w_out_d[1], b * 512, f"wout1_{b}")

        wlist = []
        for s in range(nseq):
            wlist += [(wkv_s[0], "wkv_s0"), (wkv_s[1], "wkv_s1")]
            for g in range(ngs):
                if 0 in layers:
                    wlist += [(win_s[0][b], f"win0_{b}") for b in L0_ORDER]
                if 1 in layers:
                    wlist += [(win_s[1][b], f"win1_{b}") for b in L1_ORDER]
        wstate = {"next_load": 0, "next_use": 0}

        def w_issue():
            i = wstate["next_load"]
            if i >= len(wlist):
                return
            src, key = wlist[i]
            sl = i % 3
            P.dma("sp", f"ws{sl}", lambda e: e.dma_start(out=WS[sl][:], in_=src), reads=[key], writes=[f"WS{sl}"])
            wstate["next_load"] = i + 1

        def w_next():
            i = wstate["next_use"]
            wstate["next_use"] = i + 1
            return WS[i % 3], f"WS{i % 3}"
        for _ in range(3):
            w_issue()

        def fence():
            P.pool(lambda e: e.memset(fdummy[:], 0.0), writes=["ar_all", "fdummy"])

        P.dve(lambda e: e.tensor_copy(out=posf[:], in_=posi[:]), reads=["posi"], writes=["posf"])
        def rope_compute(g):
            ang, red, r = rtmp
            P.dve(lambda e, g=g: e.tensor_tensor(
                out=ang[:], in0=posf[:, g * NCH:(g + 1) * NCH].unsqueeze(2).broadcast_to([128, NCH, 64]),
                in1=invf[:].unsqueeze(1).broadcast_to([128, NCH, 64]), op=ALU.mult),
                reads=["posf", "invf"], writes=["rt0"])
            for which, shift in ((1, 0.0), (0, math.pi / 2)):
                def f_red(e, shift=shift):
                    e.tensor_scalar(out=red[:], in0=ang[:], scalar1=shift, scalar2=1.0 / TWO_PI,
                                    op0=ALU.add, op1=ALU.mult)
                    e.tensor_scalar(out=red[:], in0=red[:], scalar1=MAGIC, scalar2=None, op0=ALU.add)
                    return e.tensor_scalar(out=red[:], in0=red[:], scalar1=-MAGIC, scalar2=-TWO_PI,
                                           op0=ALU.add, op1=ALU.mult)
                P.dve(lambda e, shift=shift: e.tensor_scalar(
                    out=red[:], in0=ang[:], scalar1=shift, scalar2=1.0 / TWO_PI, op0=ALU.add, op1=ALU.mult),
                    reads=["rt0"], writes=["rt1"])
                P.dve(lambda e: e.tensor_scalar(out=r[:], in0=red[:], scalar1=MAGIC, scalar2=None, op0=ALU.add),
                      reads=["rt1"], writes=["rt2"])
                P.dve(lambda e: e.tensor_scalar(out=red[:], in0=r[:], scalar1=-MAGIC, scalar2=-TWO_PI,
                                                op0=ALU.add, op1=ALU.mult),
                      reads=["rt2"], writes=["rt1"])
                P.dve(lambda e, shift=shift: e.scalar_tensor_tensor(
                    out=r[:], in0=ang[:], scalar=shift, in1=red[:], op0=ALU.add, op1=ALU.add),
                    reads=["rt0", "rt1"], writes=["rt2"])
                P.dve(lambda e: e.tensor_scalar(out=red[:], in0=r[:], scalar1=3.14159, scalar2=-3.14159,
                                                op0=ALU.min, op1=ALU.max),
                      reads=["rt2"], writes=["rt1"])
                P.act(lambda e, which=which: e.activation(out=rope[:, which], in_=red[:], func=AF.Sin),
                      reads=["rt1"], writes=[f"rope{which}"])
            P.act(lambda e: e.mul(out=rope[:, 2], in_=rope[:, 1], mul=-1.0), reads=["rope1"], writes=["rope2"])
            P.dma("sp", "ropest", lambda e, g=g: e.dma_start(
                out=rope_s[:, :, g * NCH:(g + 1) * NCH, :].rearrange("w p c d -> p w c d"), in_=rope[:]),
                reads=["rope0", "rope1", "rope2"], writes=[f"rope_s{g}"])

        rope_compute(0)

        def norm_transpose(src_ap, src_key, gsc_ap, gsc_key, dst_col0, tr_bank, hbi):
            ss, kss = stat(1)
            rs, krs = stat(1)
            hbt = hb[hbi]
            P.act(lambda e: e.activation(out=junk[:], in_=src_ap, func=AF.Square, accum_out=ss),
                  reads=[src_key], writes=["junk", kss])
            P.dve(lambda e: e.tensor_scalar(out=rs, in0=ss, scalar1=1.0 / D, scalar2=EPS, op0=ALU.mult, op1=ALU.add),
                  reads=[kss], writes=[krs])
            P.pool(lambda e: e.tensor_tensor(out=rs, in0=rs, in1=mhalf[:, 0:1], op=ALU.pow),
                   reads=[krs, "mhalf"], writes=[krs])
            P.act(lambda e: e.activation(out=hbt[:], in_=src_ap, func=AF.Copy, scale=rs),
                  reads=[src_key, krs], writes=[f"hb{hbi}"])
            pb = bank16(tr_bank)

            def ftr(e):
                ins = None
                for k in range(8):
                    ins = e.transpose(out=pb[:, k * 128:(k + 1) * 128], in_=hbt[:, k * 128:(k + 1) * 128],
                                      identity=ident[:])
                return ins
            P.pe(ftr, reads=[f"hb{hbi}", "ident"], writes=[f"ps{tr_bank}"])
            P.dve(lambda e: e.tensor_tensor(
                out=hT[:, :, dst_col0:dst_col0 + 128], in0=pb.rearrange("p (k t) -> p k t", k=8),
                in1=gsc_ap.unsqueeze(2).broadcast_to([128, 8, 128]), op=ALU.mult),
                reads=[f"ps{tr_bank}", gsc_key], writes=[f"hT{dst_col0 // 128}"])

        def norm_transpose_group(L):
            ss4, kss = stat(4)
            rs4, krs = stat(4)
            for c in range(NCH):
                P.act(lambda e, c=c: e.activation(out=junk[:], in_=X[:, c, :], func=AF.Square,
                                                  accum_out=ss4[:, c:c + 1]),
                      reads=[f"X{c}"], writes=["junk", kss + f"_{c}"])
            P.dve(lambda e: e.tensor_scalar(out=rs4, in0=ss4, scalar1=1.0 / D, scalar2=EPS, op0=ALU.mult, op1=ALU.add),
                  reads=[kss + f"_{c}" for c in range(NCH)], writes=[krs])
            P.pool(lambda e: e.tensor_tensor(out=rs4, in0=rs4, in1=mhalf[:, 0:4], op=ALU.pow),
                   reads=[krs, "mhalf"], writes=[krs])
            for c in range(NCH):
                hbi = c % 2
                hbt = hb[hbi]
                tr_bank = 2 + (c % 2)
                P.act(lambda e, c=c, hbt=hbt: e.activation(out=hbt[:], in_=X[:, c, :], func=AF.Copy,
                                                         scale=rs4[:, c:c + 1]),
                      reads=[f"X{c}", krs], writes=[f"hb{hbi}"])
                pb = bank16(tr_bank)

                def ftr(e, hbt=hbt, pb=pb):
                    ins = None
                    for k in range(8):
                        ins = e.transpose(out=pb[:, k * 128:(k + 1) * 128], in_=hbt[:, k * 128:(k + 1) * 128],
                                          identity=ident[:])
                    return ins
                P.pe(ftr, reads=[f"hb{hbi}", "ident"], writes=[f"ps{tr_bank}"])
                P.dve(lambda e, c=c, pb=pb: e.tensor_tensor(
                    out=hT[:, :, c * 128:(c + 1) * 128], in0=pb.rearrange("p (k t) -> p k t", k=8),
                    in1=gpre[:, L, :].unsqueeze(2).broadcast_to([128, 8, 128]), op=ALU.mult),
                    reads=[f"ps{tr_bank}", "gpre"], writes=[f"hT{c}"])

        def mm_group(out_ap, pairs, reads, wkey):
            def f(e):
                ins = None
                n = len(pairs)
                for i, (l, r) in enumerate(pairs):
                    ins = e.matmul(out_ap, lhsT=l, rhs=r, start=(i == 0), stop=(i == n - 1))
                return ins
            P.pe(f, reads=reads, writes=[wkey])

        hT_keys = [f"hT{c}" for c in range(NCH)]

        def mem_kv(s):
            for mc in range(2):
                P.dma("sp", f"xl{mc}", lambda e, mc=mc: e.dma_start(
                    out=X[:, mc, :], in_=mem_d[s * MEM + mc * 128: s * MEM + (mc + 1) * 128, :]),
                    writes=[f"X{mc}"])
            for mc in range(2):
                norm_transpose(X[:, mc, :], f"X{mc}", gmem[:], "gmem", mc * 128, mc, mc)
            w0, k0 = w_next()
            for h in range(4):
                bk = 2 + (h % 2)
                mm_group(bank(bk)[:, 0:MEM],
                         [(w0[:, k, h * 128:(h + 1) * 128], hT[:, k, 0:MEM]) for k in range(8)],
                         [k0, "hT0", "hT1"], f"ps{bk}")
                P.dve(lambda e, h=h, bk=bk: e.tensor_copy(out=memkT[:, h, :], in_=bank(bk)[:, 0:MEM]),
                      reads=[f"ps{bk}"], writes=["memkT"])
            w_issue()
            w1, k1 = w_next()
            for mc in range(2):
                bk = 4 + mc
                mm_group(bank(bk), [(hT[:, k, mc * 128:(mc + 1) * 128], w1[:, k, :]) for k in range(8)],
                         [k1, f"hT{mc}"], f"ps{bk}")
                P.act(lambda e, mc=mc, bk=bk: e.activation(out=memv[:, mc, :], in_=bank(bk), func=AF.Copy),
                      reads=[f"ps{bk}"], writes=["memv"])
            w_issue()

        def mem_attn():
            sc = 128.0 ** -0.5
            for hd in range(4):
                par = hd % 2
                b0 = 4 * par
                pts = [pT[2 * par + mc] for mc in range(2)]
                ptk = [f"pT{2 * par + mc}" for mc in range(2)]
                ri, xt = rinv2[par], xtmp2[par]
                for mc in range(2):
                    bk = b0 + mc
                    mm_group(bank(bk), [(memkT[:, hd, mc * 128:(mc + 1) * 128], xqT[:, hd, :])],
                             ["memkT", "xqT"], f"ps{bk}")
                    P.act(lambda e, mc=mc, bk=bk, pts=pts: e.activation(out=pts[mc][:], in_=bank(bk), func=AF.Exp, scale=sc),
                          reads=[f"ps{bk}"], writes=[ptk[mc]])
                mm_group(bank(b0 + 2), [(memv[:, mc, hd * 128:(hd + 1) * 128], pts[mc][:]) for mc in range(2)],
                         ["memv"] + ptk, f"ps{b0 + 2}")
                mm_group(bank(b0 + 3), [(ones[:], pts[mc][:]) for mc in range(2)], ["ones"] + ptk, f"ps{b0 + 3}")
                P.dve(lambda e, ri=ri, b0=b0: e.reciprocal(out=ri[:], in_=bank(b0 + 3)), reads=[f"ps{b0 + 3}"],
                      writes=[f"rinv{par}"])
                P.dve(lambda e, ri=ri, xt=xt, b0=b0: e.tensor_tensor(out=xt[:], in0=bank(b0 + 2), in1=ri[:], op=ALU.mult),
                      reads=[f"ps{b0 + 2}", f"rinv{par}"], writes=[f"xtmp{par}"])
                P.dve(lambda e, hd=hd, xt=xt: e.tensor_tensor(out=yT[:, 12 + hd, :], in0=xt[:], in1=yT[:, 12 + hd, :],
                                                              op=ALU.mult),
                      reads=[f"xtmp{par}", f"yT{12 + hd}"], writes=[f"yT{12 + hd}"])

        def wo_prefetch(L):
            if L == 0:
                dead = [[f"Q{c}" for c in range(NCH)] + [f"K{c}" for c in range(NCH)],
                        [f"V{c}" for c in range(NCH)] + ["qT0", "qT1"]]
            else:
                dead = [[f"conv{cb}" for cb in range(8)],
                        [f"conv{cb}" for cb in range(8, 12)] + ["sig0", "sig1", "yb0", "yb1", "ysq0", "ysq1", "MEAN"]]
            for nb in range(2):
                P.dma("sp", f"wo{nb}", lambda e, nb=nb: e.dma_start(out=Wo[nb], in_=wout_s[L][nb]),
                      reads=[f"wout{L}_{nb}"], writes=[f"Wo{nb}"] + dead[nb])

        def out_proj(L, tok0, last_layer):
            fence()
            ykeys = [f"yT{m}" for m in range(16)]
            for c in range(NCH):
                b0 = 4 + 2 * (c % 2)
                for nb in range(2):
                    mm_group(bank(b0 + nb), [(yT[:, m, c * 128:(c + 1) * 128], Wo[nb][:, m, :]) for m in range(16)],
                             ykeys + [f"Wo{nb}"] + ARENA_KEYS, f"ps{b0 + nb}")
                ss2, kss2 = stat(2)
                rs, krs = stat(1)
                for nb in range(2):
                    P.act(lambda e, nb=nb, ss2=ss2, b0=b0: e.activation(out=junk[:, 0:512], in_=bank(b0 + nb), func=AF.Square,
                                                                 accum_out=ss2[:, nb:nb + 1]),
                          reads=[f"ps{b0 + nb}"], writes=["junk", kss2 + f"_{nb}"])
                P.dve(lambda e, ss2=ss2, rs=rs: e.tensor_tensor(out=rs, in0=ss2[:, 0:1], in1=ss2[:, 1:2], op=ALU.add),
                      reads=[kss2 + "_0", kss2 + "_1"], writes=[krs])
                P.dve(lambda e, rs=rs: e.tensor_scalar(out=rs, in0=rs, scalar1=1.0 / D, scalar2=EPS,
                                                      op0=ALU.mult, op1=ALU.add),
                      reads=[krs], writes=[krs])
                P.pool(lambda e, rs=rs: e.tensor_tensor(out=rs, in0=rs, in1=mhalf[:, 0:1], op=ALU.pow),
                       reads=[krs, "mhalf"], writes=[krs])
                P.dve(lambda e, b0=b0: e.tensor_tensor(
                    out=otmp, in0=psum[:, b0:b0 + 2, :].rearrange("p a b -> p (a b)"), in1=gpost[:, L, :], op=ALU.mult),
                    reads=[f"ps{b0}", f"ps{b0 + 1}", "gpost"] + ARENA_KEYS, writes=["otmp"])
                P.dve(lambda e, c=c, rs=rs: e.scalar_tensor_tensor(
                    out=X[:, c, :], in0=otmp, scalar=rs, in1=X[:, c, :], op0=ALU.mult, op1=ALU.add),
                    reads=["otmp", krs, f"X{c}"], writes=[f"X{c}"])
                if last_layer:
                    P.dma("sp", f"xs{c}", lambda e, c=c: e.dma_start(
                        out=out_d[tok0 + c * 128: tok0 + (c + 1) * 128, :], in_=X[:, c, :]),
                        reads=[f"X{c}"])
            fence()

        IPB = [0, 1, 4, 5, 6, 7]

        def feature_major_blocks(nblk, first_mc, silu):
            for b in range(nblk):
                w, wk = w_next()
                for s in range(4):
                    bk = IPB[(b * 4 + s) % len(IPB)]
                    mm_group(bank(bk), [(w[:, k, s * 128:(s + 1) * 128], hT[:, k, :]) for k in range(8)],
                             [wk] + hT_keys, f"ps{bk}")
                    mc = first_mc + 4 * b + s
                    if silu:
                        P.act(lambda e, mc=mc, bk=bk: e.activation(out=yT[:, mc, :], in_=bank(bk), func=AF.Silu),
                              reads=[f"ps{bk}"], writes=[f"yT{mc}"])
                    else:
                        P.act(lambda e, mc=mc, bk=bk: e.activation(out=xqT[:, mc, :], in_=bank(bk), func=AF.Copy),
                              reads=[f"ps{bk}"], writes=["xqT"])
                w_issue()

        def layer0(gi, gs, tok0):
            if DBG_STAGE < 1:
                return
            norm_transpose_group(0)
            if DBG_STAGE < 2:
                return
            cosb = rope[:, 0]
            sinb = rope[:, 1]
            nsinb = rope[:, 2]
            for blk in range(4):
                w, wk = w_next()
                dst = Qb if blk < 2 else Kb
                dkey = "Q" if blk < 2 else "K"
                col0 = (blk % 2) * 512
                for c in range(NCH):
                    bk = IPB[(blk * NCH + c) % len(IPB)]
                    mm_group(bank(bk), [(hT[:, k, c * 128:(c + 1) * 128], w[:, k, :]) for k in range(8)],
                             [wk, f"hT{c}"] + ARENA_KEYS, f"ps{bk}")
                    t1 = t1b[c % 2]
                    t2 = t2b[c % 2]
                    p4 = bank(bk).rearrange("p (h two d) -> p h two d", h=4, two=2)
                    t14 = t1.rearrange("p (h two d) -> p h two d", h=4, two=2)
                    t24 = t2.rearrange("p (h two d) -> p h two d", h=4, two=2)
                    P.dve(lambda e, c=c, p4=p4, t14=t14: e.tensor_tensor(
                        out=t14, in0=p4, in1=cosb[:, c, :].unsqueeze(1).unsqueeze(1).broadcast_to([128, 4, 2, 64]),
                        op=ALU.mult), reads=[f"ps{bk}", "rope"] + ARENA_KEYS, writes=[f"t1_{c % 2}"])

                    def frot(e, c=c, p4=p4, t24=t24):
                        e.tensor_tensor(out=t24[:, :, 0, :], in0=p4[:, :, 1, :],
                                        in1=nsinb[:, c, :].unsqueeze(1).broadcast_to([128, 4, 64]), op=ALU.mult)
                        return e.tensor_tensor(out=t24[:, :, 1, :], in0=p4[:, :, 0, :],
                                               in1=sinb[:, c, :].unsqueeze(1).broadcast_to([128, 4, 64]), op=ALU.mult)
                    P.dve(frot, reads=[f"ps{bk}", "rope"] + ARENA_KEYS, writes=[f"t2_{c % 2}"])
                    P.pool(lambda e, c=c, t1=t1, t2=t2, dst=dst, col0=col0: e.tensor_tensor(
                        out=dst[:, c, col0:col0 + 512], in0=t1, in1=t2, op=ALU.add),
                        reads=[f"t1_{c % 2}", f"t2_{c % 2}"] + ARENA_KEYS, writes=[f"{dkey}{c}"])
                w_issue()
            if DBG_STAGE < 3:
                return
            for vb in range(3):
                w, wk = w_next()
                for c in range(NCH):
                    bk = IPB[(vb * NCH + c) % len(IPB)]
                    mm_group(bank(bk), [(hT[:, k, c * 128:(c + 1) * 128], w[:, k, :]) for k in range(8)],
                             [wk, f"hT{c}"], f"ps{bk}")

                    def fv(e, c=c, bk=bk, vb=vb):
                        ins = None
                        for h in range(NH):
                            lo = max(DV * h, 512 * vb)
                            hi = min(DV * h + DV, 512 * vb + 512)
                            if lo >= hi:
                                continue
                            ins = e.activation(out=Vb[:, c, lo:hi], in_=bank(bk)[:, lo - 512 * vb: hi - 512 * vb],
                                               func=AF.Copy, scale=zs[:, h:h + 1])
                        return ins
                    P.act(fv, reads=[f"ps{bk}", "zs"] + ARENA_KEYS, writes=[f"V{c}"])
                w_issue()
            if DBG_STAGE < 4:
                return
            feature_major_blocks(1, 0, False)
            feature_major_blocks(4, 0, True)
            if DBG_STAGE < 5:
                return
            if gs == 0:
                P.dve(lambda e: e.memset(state[:], 0.0), writes=["state0", "state1"])
                P.pool(lambda e: e.memset(state_bf[:], 0.0), writes=["stbf0", "stbf1"])
            def head(c):
                bi = c % 2
                for (src, skey, dstT, dkey, bk, scaled) in ((Qb, "Q", qT[bi], f"qT{bi}", 0, True),
                                                            (Kb, "K", kT[bi], f"kT{bi}", 1, False)):
                    pb = bank16(bk)

                    def ftr(e, src=src, pb=pb, c=c):
                        ins = None
                        for h in range(NH):
                            ins = e.transpose(out=pb[:, h * 128:(h + 1) * 128], in_=src[:, c, h * 128:(h + 1) * 128],
                                              identity=ident[:])
                        return ins
                    P.pe(ftr, reads=[f"{skey}{c}", "ident"] + ARENA_KEYS, writes=[f"ps{bk}"])
                    if scaled:
                        P.dve(lambda e, pb=pb, dstT=dstT: e.tensor_tensor(
                            out=dstT, in0=pb.rearrange("p (h t) -> p h t", h=NH), in1=xi[:], op=ALU.mult),
                            reads=[f"ps{bk}", "xi"] + ARENA_KEYS, writes=[dkey])
                    else:
                        P.act(lambda e, pb=pb, dstT=dstT: e.activation(
                            out=dstT, in_=pb.rearrange("p (h t) -> p h t", h=NH), func=AF.Copy),
                            reads=[f"ps{bk}"] + ARENA_KEYS, writes=[dkey])
            def halves(c, hh_list, phase="AB"):
                bi = c % 2
                for hh in hh_list:
                    hs = list(range(4 * hh, 4 * hh + 4))
                    ST = STb[2 * bi + hh]
                    skey = f"ST{2 * bi + hh}"
                    ob = 3 if hh == 0 else 5
                    okeys = [f"ps{ob}", f"ps{ob + 1}"]
                    if "A" in phase:
                        def fs(e, hs=hs, bi=bi):
                            ins = None
                            for n, h in enumerate(hs):
                                ins = e.matmul(bank(2)[:, n * 128:(n + 1) * 128], lhsT=kT[bi][:, h, :], rhs=qT[bi][:, h, :],
                                               start=True, stop=True)
                            return ins
                        P.pe(fs, reads=[f"qT{bi}", f"kT{bi}"] + ARENA_KEYS, writes=["ps2"])
                        P.dve(lambda e, ST=ST, hh=hh: e.tensor_tensor(
                            out=ST, in0=bank(2).rearrange("p (h t) -> p h t", h=4), in1=mask[:, 4 * hh:4 * hh + 4, :],
                            op=ALU.mult), reads=["ps2", "mask"] + ARENA_KEYS, writes=[skey])
                        if DBG_STAGE < 4.2:
                            continue

                        def fo(e, hs=hs, bi=bi, ST=ST, c=c, ob=ob):
                            ins = None
                            for n, h in enumerate(hs):
                                o = bank(ob + n // 2)[:, (n % 2) * DV:(n % 2) * DV + DV]
                                e.matmul(o, lhsT=ST[:, n, :], rhs=Vb[:, c, h * DV:(h + 1) * DV], start=True, stop=False)
                                ins = e.matmul(o, lhsT=qT[bi][:, h, :], rhs=state_bf[:, h, :], start=False, stop=True)
                            return ins
                        P.pe(fo, reads=[skey, f"V{c}", f"qT{bi}", f"stbf{hh}"] + ARENA_KEYS, writes=okeys)
                        if DBG_STAGE < 4.3:
                            continue
                        last_chunk = False
                        if not last_chunk:
                            for pp in range(2):
                                def fd(e, hs=hs, c=c, pp=pp):
                                    ins = None
                                    for n in (2 * pp, 2 * pp + 1):
                                        h = hs[n]
                                        o = bank(7)[:, (n % 2) * DV:(n % 2) * DV + DV]
                                        ins = e.matmul(o, lhsT=Kb[:, c, h * 128:(h + 1) * 128],
                                                       rhs=Vb[:, c, h * DV:(h + 1) * DV], start=True, stop=True)
                                    return ins
                                P.pe(fd, reads=[f"K{c}", f"V{c}"] + ARENA_KEYS, writes=["ps7"])

                                def fst(e, hs=hs, pp=pp):
                                    ins = None
                                    for n in (2 * pp, 2 * pp + 1):
                                        h = hs[n]
                                        o = bank(7)[:, (n % 2) * DV:(n % 2) * DV + DV]
                                        ins = e.scalar_tensor_tensor(out=state[:, h, :], in0=state[:, h, :], scalar=cdec[h],
                                                                     in1=o, op0=ALU.mult, op1=ALU.add)
                                    return ins
                                P.dve(fst, reads=["ps7", f"state{hh}"], writes=[f"state{hh}"])
                            P.act(lambda e, hh=hh: e.activation(out=state_bf[:, 4 * hh:4 * hh + 4, :],
                                                               in_=state[:, 4 * hh:4 * hh + 4, :], func=AF.Copy),
                                  reads=[f"state{hh}"], writes=[f"stbf{hh}"])
                    if "B" in phase:
                        if DBG_STAGE < 4.4 or "stats" in SKIP:
                            continue
                        o4 = psum[:, ob:ob + 2, 0:2 * DV].rearrange("p b (h v) -> p b h v", h=2)
                        sm, ksm = stat(4)
                        sq, ksq = stat(4)
                        mu, kmu = stat(4)
                        rs, krs = stat(4)
                        nm, knm = stat(4)
                        sqs = t1b[0].rearrange("p (b h v) -> p b h v", b=2, h=2)[:, :, :, 0:DV] if False else None
                        P.dve(lambda e, sm=sm, o4=o4: e.tensor_reduce(
                            out=sm.rearrange("p (b h) -> p b h", b=2), in_=o4, axis=AX.X, op=ALU.add),
                            reads=okeys, writes=[ksm])

                        def fsq(e, sq=sq, ob=ob):
                            ins = None
                            for n in range(4):
                                o = bank(ob + n // 2)[:, (n % 2) * DV:(n % 2) * DV + DV]
                                ins = e.activation(out=junk[:, n * DV:(n + 1) * DV], in_=o, func=AF.Square, accum_out=sq[:, n:n + 1])
                            return ins
                        P.act(fsq, reads=okeys, writes=["junk", ksq])
                        P.dve(lambda e, sm=sm, mu=mu: e.tensor_scalar(out=mu, in0=sm, scalar1=1.0 / DV, scalar2=None,
                                                                     op0=ALU.mult), reads=[ksm], writes=[kmu])
                        P.dve(lambda e, mu=mu, nm=nm: e.tensor_tensor(out=nm, in0=mu, in1=mu, op=ALU.mult),
                              reads=[kmu], writes=[knm])
                        P.dve(lambda e, sq=sq, nm=nm, rs=rs: e.scalar_tensor_tensor(
                            out=rs, in0=sq, scalar=1.0 / DV, in1=nm, op0=ALU.mult, op1=ALU.subtract),
                            reads=[ksq, knm], writes=[krs])
                        P.dve(lambda e, rs=rs: e.tensor_scalar(out=rs, in0=rs, scalar1=EPS, scalar2=None, op0=ALU.add),
                              reads=[krs], writes=[krs])
                        P.pool(lambda e, rs=rs: e.tensor_tensor(out=rs, in0=rs, in1=mhalf[:, 0:4],
                                                                op=ALU.pow), reads=[krs, "mhalf"], writes=[krs])
                        P.dve(lambda e, mu=mu, rs=rs, nm=nm: e.scalar_tensor_tensor(
                            out=nm, in0=mu, scalar=-1.0, in1=rs, op0=ALU.mult, op1=ALU.mult),
                            reads=[kmu, krs], writes=[knm])
                        if DBG_STAGE < 4.5 or "fap" in SKIP:
                            continue
                        ON = ONb[bi]

                        def fap(e, hs=hs, rs=rs, nm=nm, ON=ON, ob=ob):
                            ins = None
                            for n, h in enumerate(hs):
                                o = bank(ob + n // 2)[:, (n % 2) * DV:(n % 2) * DV + DV]
                                ins = e.activation(out=ON[:, h * DV:(h + 1) * DV], in_=o, func=AF.Identity,
                                                   scale=rs[:, n:n + 1], bias=nm[:, n:n + 1])
                            return ins
                        P.act(fap, reads=okeys + [krs, knm] + ARENA_KEYS, writes=[f"ON{bi}_{hh}"])

            def tail(c):
                bi = c % 2
                ON = ONb[bi]
                for p_ in range(2):
                    m0, m1 = 6 * p_, 6 * p_ + 6

                    def ftr2(e, ON=ON, m0=m0, m1=m1):
                        ins = None
                        for m in range(m0, m1):
                            ins = e.transpose(out=bank16(2)[:, (m - m0) * 128:(m - m0 + 1) * 128],
                                              in_=ON[:, m * 128:(m + 1) * 128], identity=ident[:])
                        return ins
                    P.pe(ftr2, reads=[f"ON{bi}_0", f"ON{bi}_1", "ident"], writes=["ps2"])
                    tmpb = ontT[:, m0:m1, :]
                    P.act(lambda e, tmpb=tmpb: e.activation(
                        out=tmpb, in_=bank16(2)[:, 0:6 * 128].rearrange("p (m t) -> p m t", m=6), func=AF.Copy),
                        reads=["ps2"], writes=[f"ontT{p_}"])
                    P.dve(lambda e, c=c, m0=m0, m1=m1, tmpb=tmpb: e.tensor_tensor(
                        out=yT[:, m0:m1, c * 128:(c + 1) * 128], in0=tmpb,
                        in1=yT[:, m0:m1, c * 128:(c + 1) * 128], op=ALU.mult),
                        reads=[f"ontT{p_}"] + [f"yT{m}" for m in range(m0, m1)],
                        writes=[f"yT{m}" for m in range(m0, m1)])

            head(0)
            halves(0, [0, 1], "A")
            halves(0, [0, 1], "B")
            for c in range(1, NCH):
                head(c)
                halves(c, [0, 1], "A")
                tail(c - 1)
                halves(c, [0, 1], "B")
            tail(NCH - 1)
            if "pad" in SKIP:
                for i in range(60):
                    if "padscale" in SKIP:
                        P.act(lambda e: e.activation(out=xtmp[:, 0:16], in_=rinv[:, 0:16], func=AF.Copy, scale=zs[:, 0:1]),
                              reads=["rinv", "zs"], writes=["xtmp"])
                    elif "padbias" in SKIP:
                        P.act(lambda e: e.activation(out=xtmp[:, 0:16], in_=rinv[:, 0:16], func=AF.Identity, bias=zs[:, 0:1]),
                              reads=["rinv", "zs"], writes=["xtmp"])
                    else:
                        P.act(lambda e: e.activation(out=xtmp[:, 0:16], in_=rinv[:, 0:16], func=AF.Copy),
                              reads=["rinv"], writes=["xtmp"])
            if DBG_STAGE < 6:
                return
            wo_prefetch(0)
            mem_attn()
            if DBG_STAGE < 7:
                return
            out_proj(0, tok0, last_layer=(1 not in layers))

        def layer1(gi, gs, tok0):
            norm_transpose_group(1)
            if gs == 0:
                P.pool(lambda e: e.memset(gluT[:, :, 0:30], 0.0), writes=["gluhist"])
                P.pool(lambda e: e.memset(gluT[:, :, T + 30:T + 32], 0.0), writes=["glupad"])
            for i in range(3):
                wu, ku = w_next()
                wg, kg = w_next()
                for s in range(4):
                    cb = 4 * i + s
                    ba = 2 * (s % 2)
                    mm_group(bank(ba), [(wu[:, k, s * 128:(s + 1) * 128], hT[:, k, :]) for k in range(8)],
                             [ku] + hT_keys, f"ps{ba}")
                    mm_group(bank(ba + 1), [(wg[:, k, s * 128:(s + 1) * 128], hT[:, k, :]) for k in range(8)],
                             [kg] + hT_keys, f"ps{ba + 1}")
                    sg = sigb[s % 2]
                    P.act(lambda e, sg=sg, ba=ba: e.activation(out=sg, in_=bank(ba + 1), func=AF.Sigmoid),
                          reads=[f"ps{ba + 1}"] + ARENA_KEYS, writes=[f"sig{s % 2}"])
                    P.dve(lambda e, sg=sg, ba=ba, cb=cb: e.tensor_tensor(
                        out=gluT[:, cb, 30:30 + T], in0=bank(ba), in1=sg, op=ALU.mult),
                        reads=[f"ps{ba}", f"sig{s % 2}"] + ARENA_KEYS, writes=[f"glu{cb}"])
                w_issue()
                w_issue()
            feature_major_blocks(1, 0, False)
            feature_major_blocks(4, 0, True)
            def build_b(cb):
                dB = diagB2[cb % 2]

                def fdb(e, cb=cb, dB=dB):
                    ins = None
                    for j in range(15):
                        ins = e.activation(out=dB[:, j, :], in_=ident[:], func=AF.Copy, scale=dw[:, cb, 16 + j:17 + j])
                    return ins
                P.act(fdb, reads=["ident", "dw"], writes=[f"diagB{cb % 2}"])

            build_b(0)
            for cb in range(12):
                bk = 6 + (cb % 2)
                dB = diagB2[cb % 2]

                def fda(e, cb=cb):
                    ins = None
                    for j in range(16):
                        ins = e.tensor_scalar(out=diagA[:, j, :], in0=identf[:], scalar1=dw[:, cb, j:j + 1], scalar2=None,
                                              op0=ALU.mult)
                    return ins
                P.dve(fda, reads=["identf", "dw"], writes=["diagA"])
                if cb + 1 < 12:
                    build_b(cb + 1)

                def fca(e, cb=cb, bk=bk):
                    ins = None
                    for j in range(16):
                        ins = e.matmul(bank(bk), lhsT=diagA[:, j, :], rhs=gluT[:, cb, j:j + T], start=(j == 0), stop=False)
                    return ins
                P.pe(fca, reads=["diagA", f"glu{cb}", "gluhist"], writes=[f"ps{bk}"])

                def fcb(e, cb=cb, bk=bk, dB=dB):
                    ins = None
                    for j in range(15):
                        k = 16 + j
                        ins = e.matmul(bank(bk), lhsT=dB[:, j, :], rhs=gluT[:, cb, k:k + T], start=False, stop=(j == 14))
                    return ins
                P.pe(fcb, reads=[f"diagB{cb % 2}", f"glu{cb}", "gluhist"], writes=[f"ps{bk}"])
                P.act(lambda e, cb=cb, bk=bk: e.activation(out=convT[:, cb, :], in_=bank(bk), func=AF.Identity,
                                                           bias=dwb[:, cb:cb + 1]),
                      reads=[f"ps{bk}", "dwb"], writes=[f"conv{cb}"])
            P.act(lambda e: e.activation(out=gluT[:, :, 0:30], in_=gluT[:, :, T:T + 30], func=AF.Copy),
                  reads=[f"glu{cb}" for cb in range(12)] + ["gluhist"], writes=["gluhist"])
            for cb in range(12):
                yb = ybb[cb % 2]
                ysq = ysqb[cb % 2]
                P.act(lambda e, cb=cb, yb=yb: e.activation(out=yb, in_=convT[:, cb, :], func=AF.Copy),
                      reads=[f"conv{cb}"], writes=[f"yb{cb % 2}"])
                P.act(lambda e, cb=cb, ysq=ysq: e.activation(out=ysq, in_=convT[:, cb, :], func=AF.Square),
                      reads=[f"conv{cb}"], writes=[f"ysq{cb % 2}"])
                P.pe(lambda e, cb=cb, yb=yb: e.matmul(bank(4), lhsT=ones[:], rhs=yb, start=(cb == 0), stop=(cb == 11)),
                     reads=["ones", f"yb{cb % 2}"], writes=["ps4"])
                P.pe(lambda e, cb=cb, ysq=ysq: e.matmul(bank(5), lhsT=ones[:], rhs=ysq, start=(cb == 0),
                                                        stop=(cb == 11)),
                     reads=["ones", f"ysq{cb % 2}"], writes=["ps5"])
            P.act(lambda e: e.activation(out=MEAN, in_=bank(4), func=AF.Copy, scale=1.0 / BW),
                  reads=["ps4"], writes=["MEAN"])
            P.dve(lambda e: e.tensor_tensor(out=VAR, in0=MEAN, in1=MEAN, op=ALU.mult), reads=["MEAN"], writes=["VAR"])
            P.dve(lambda e: e.scalar_tensor_tensor(out=RSTD, in0=bank(5), scalar=1.0 / BW, in1=VAR,
                                                   op0=ALU.mult, op1=ALU.subtract),
                  reads=["ps5", "VAR"], writes=["RSTD"])
            P.dve(lambda e: e.tensor_scalar(out=VAR, in0=RSTD, scalar1=EPS, scalar2=None, op0=ALU.add),
                  reads=["RSTD"], writes=["VAR"])
            P.dve(lambda e: e.reciprocal(out=VAR, in_=VAR), reads=["VAR"], writes=["VAR"])
            P.act(lambda e: e.activation(out=RSTD, in_=VAR, func=AF.Sqrt), reads=["VAR"], writes=["RSTD"])
            P.dve(lambda e: e.scalar_tensor_tensor(out=NMR, in0=MEAN, scalar=-1.0, in1=RSTD, op0=ALU.mult, op1=ALU.mult),
                  reads=["MEAN", "RSTD"], writes=["NMR"])
            for cb in range(12):
                z = zb[cb % 2]
                a = ab[cb % 2]
                P.dve(lambda e, cb=cb, z=z: e.tensor_tensor(out=z, in0=convT[:, cb, :], in1=RSTD, op=ALU.mult),
                      reads=[f"conv{cb}", "RSTD"], writes=[f"z{cb % 2}"])
                P.dve(lambda e, z=z: e.tensor_tensor(out=z, in0=z, in1=NMR, op=ALU.add),
                      reads=[f"z{cb % 2}", "NMR"], writes=[f"z{cb % 2}"])
                P.act(lambda e, cb=cb, z=z, a=a: e.activation(out=a, in_=z, func=AF.Silu, scale=lng[:, cb:cb + 1],
                                                             bias=lnb[:, cb:cb + 1]),
                      reads=[f"z{cb % 2}", "lng", "lnb"], writes=[f"a{cb % 2}"])
                P.dve(lambda e, cb=cb, a=a: e.tensor_tensor(out=yT[:, cb, :], in0=a, in1=yT[:, cb, :], op=ALU.mult),
                      reads=[f"a{cb % 2}", f"yT{cb}"], writes=[f"yT{cb}"])
            wo_prefetch(1)
            mem_attn()
            out_proj(1, tok0, last_layer=True)

        for s in range(nseq):
            mem_kv(s)
            for gs in range(ngs):
                gi = s * ngs + gs
                tok0 = s * seqlen + gs * T
                for c in range(NCH):
                    P.dma("sp", f"xl{c}", lambda e, c=c, tok0=tok0: e.dma_start(
                        out=X[:, c, :], in_=x_d[tok0 + c * 128: tok0 + (c + 1) * 128, :]), writes=[f"X{c}"])
                P.dma("sp", "ropeld", lambda e, gi=gi: e.dma_start(
                    out=rope[:], in_=rope_s[:, :, gi * NCH:(gi + 1) * NCH, :].rearrange("w p c d -> p w c d")),
                    reads=[f"rope_s{gi}"], writes=["rope", "rope0", "rope1", "rope2"])
                if 0 in layers:
                    layer0(gi, gs, tok0)
                if gi + 1 < ng:
                    rope_compute(gi + 1)
                if 1 in layers:
                    layer1(gi, gs, tok0)
        with nc.allow_low_precision("bf16 matmul operands, fp32 accumulation"):
            P.finish()
        build_program.last_prog = P
    return nc


def _core_inputs(i, nseq, inputs, consts):
    f = np.float32
    sl = slice(i * nseq, (i + 1) * nseq)
    x = np.ascontiguousarray(inputs["x"][sl]).reshape(-1, D)
    mem = np.ascontiguousarray(inputs["mem"][sl]).reshape(-1, D)
    pos = np.ascontiguousarray(inputs["positions"][sl]).reshape(-1)
    ncht = pos.shape[0] // 128
    m = {
        "x": x, "mem": mem,
        "pos": np.ascontiguousarray(pos.reshape(ncht, 128).T).astype(np.int32),
        "w_mem_kv": inputs["w_mem_kv"],
        "ret_w_in": inputs["ret_w_in"][0], "conv_w_in": inputs["conv_w_in"][0],
        "ret_w_out": inputs["ret_w_out"][0], "conv_w_out": inputs["conv_w_out"][0],
        "g_mem": np.ascontiguousarray(inputs["mem_norm_g"].reshape(8, 128).T),
        "g_pre": np.ascontiguousarray(inputs["norm_pre_g"].reshape(2, 8, 128).transpose(2, 0, 1)),
        "g_post": np.ascontiguousarray(np.broadcast_to(inputs["norm_post_g"][None], (128, 2, D))),
        "dw_w": np.ascontiguousarray(inputs["conv_dw_w"][0].reshape(CW, 12, 128).transpose(2, 1, 0)),
        "dw_b": np.ascontiguousarray(inputs["conv_dw_b"][0].reshape(12, 128).T),
        "ln_g": np.ascontiguousarray(inputs["conv_ln_g"][0].reshape(12, 128).T),
        "ln_b": np.ascontiguousarray(inputs["conv_ln_b"][0].reshape(12, 128).T),
    }
    m.update(consts)
    return {k: np.ascontiguousarray(v) for k, v in m.items()}


_CACHE = {}


def run(inputs, n_cores, nseq, seqlen, layers=(0, 1), trace=False):
    key = (nseq, seqlen, tuple(layers))
    if key not in _CACHE:
        _CACHE[key] = build_program(nseq, seqlen, layers)
    nc = _CACHE[key]
    consts, _ = make_consts()
    inputs = {k: np.asarray(v) for k, v in inputs.items()}
    in_maps = [_core_inputs(i, nseq, inputs, consts) for i in range(n_cores)]
    res = run_bass_kernel_spmd(nc, in_maps, core_ids=list(range(n_cores)), **({"trace": True} if trace else {}))
    out = np.stack([r["out"].reshape(nseq, seqlen, D) for r in res.results], axis=0)
    return out.reshape(n_cores * nseq, seqlen, D), res


def kernel(x, mem, positions, mem_norm_g, w_mem_kv, norm_pre_g, norm_post_g, ret_w_in, ret_w_out,
           conv_w_in, conv_dw_w, conv_dw_b, conv_ln_g, conv_ln_b, conv_w_out):
    inputs = dict(x=x, mem=mem, positions=positions, mem_norm_g=mem_norm_g, w_mem_kv=w_mem_kv,
                  norm_pre_g=norm_pre_g, norm_post_g=norm_post_g, ret_w_in=ret_w_in, ret_w_out=ret_w_out,
                  conv_w_in=conv_w_in, conv_dw_w=conv_dw_w, conv_dw_b=conv_dw_b, conv_ln_g=conv_ln_g,
                  conv_ln_b=conv_ln_b, conv_w_out=conv_w_out)
    out, _ = run(inputs, 8, 2, 2048)
    return out.astype(np.float32)
```

```python
import math
import re
import numpy as np
from contextlib import ExitStack
import concourse.bass as bass
import concourse.mybir as mybir
from concourse.bass_utils import run_bass_kernel_spmd

F32 = mybir.dt.float32
BF16 = mybir.dt.bfloat16
I32 = mybir.dt.int32
AF = mybir.ActivationFunctionType
ALU = mybir.AluOpType
AX = mybir.AxisListType

D = 1024
MEM = 256
EPS = 1e-6
NH = 8
DV = 192
BW = 1536
CW = 31
NCH = 4
T = NCH * 128
RET_IN = 6144
CONV_IN = 5632
MAGIC = 12582912.0
DBG_STAGE = 99
SKIP = set()
TWO_PI = 2.0 * math.pi


PS_RE = re.compile(r"^ps\d$")
ARENA_RE = re.compile(r"^(diag|Q\d|K\d|V\d|VAR|qT|kT|ST\d|ON|t1_|t2_|Wo|otmp|conv|sig|yb|ysq|MEAN|RSTD|NMR|z\d|a\d)")


class Prog:
    ENGS = ("pe", "dve", "act", "pool", "sp")

    def __init__(self, nc, stack):
        self.nc = nc
        self.stack = stack
        self.ops = []
        self.last_w = {}
        self.readers = {}

    def sb(self, name, shape, dtype):
        return self.stack.enter_context(self.nc.sbuf_tensor(name, list(shape), dtype))

    def ps(self, name, shape, dtype):
        return self.stack.enter_context(self.nc.psum_tensor(name, list(shape), dtype))

    def op(self, eng, fn, reads=(), writes=(), dma=None, ndma=1):
        idx = len(self.ops)
        deps = set()
        is_dma = dma is not None
        reads = list(reads)
        writes = list(writes)
        if "ar_all" not in writes and any(ARENA_RE.match(k) for k in reads + writes):
            reads.append("ar_all")
        for k in reads:
            if PS_RE.match(k) and k not in writes:
                writes.append(k)

        def add(d, raw):
            if d is None or d == idx:
                return
            od = self.ops[d]
            deps.add(d)
        for r in reads:
            add(self.last_w.get(r), True)
        for w in writes:
            add(self.last_w.get(w), False)
            for rd in self.readers.get(w, ()):
                add(rd, False)
        for r in reads:
            self.readers.setdefault(r, []).append(idx)
        for w in writes:
            self.last_w[w] = idx
            self.readers[w] = []
        import sys as _sys
        fr = _sys._getframe(1)
        while fr.f_code.co_name in ('op', 'pe', 'dve', 'act', 'pool', 'dma', 'mm_group'):
            fr = fr.f_back
        self.ops.append(dict(eng=eng, fn=fn, deps=deps, dma=dma, ndma=ndma, line=fr.f_lineno))
        return idx

    def pe(self, fn, reads=(), writes=()):
        return self.op("pe", fn, reads, writes)

    def dve(self, fn, reads=(), writes=()):
        return self.op("dve", fn, reads, writes)

    def act(self, fn, reads=(), writes=()):
        return self.op("act", fn, reads, writes)

    def pool(self, fn, reads=(), writes=()):
        return self.op("pool", fn, reads, writes)

    def dma(self, queue, semname, fn, reads=(), writes=(), ndma=1):
        return self.op(queue, fn, reads, writes, dma=semname, ndma=ndma)

    def finish(self, final_wait_eng="sp"):
        nc = self.nc
        ops = self.ops
        n = len(ops)
        needed = set()
        for o in ops:
            needed |= o["deps"]
        sem_names = set(self.ENGS)
        for o in ops:
            if o["dma"] is not None:
                sem_names.add("dma:" + o["dma"])
        sems = {}
        for s in sorted(sem_names):
            sems[s] = self.stack.enter_context(nc.semaphore(s.replace(":", "_")))
        counts = {s: 0 for s in sem_names}
        ev = [None] * n
        vc = [None] * n
        clock = {e: {} for e in self.ENGS}
        streams = {e: [] for e in self.ENGS}
        outstanding = {}
        for i, o in enumerate(ops):
            e = o["eng"]
            ck = clock[e]
            waits = {}
            for d in sorted(o["deps"]):
                s, v = ev[d]
                if ck.get(s, 0) >= v:
                    continue
                if waits.get(s, 0) < v:
                    waits[s] = v
                for s2, v2 in vc[d].items():
                    if ck.get(s2, 0) < v2:
                        ck[s2] = v2
            if o["dma"] is not None:
                s = "dma:" + o["dma"]
                counts[s] += 16 * o["ndma"]
                ev[i] = (s, counts[s])
                inc = (s, 16)
                outstanding[s] = counts[s]
                v = dict(ck)
                v[s] = counts[s]
                vc[i] = v
            elif i in needed:
                counts[e] += 1
                ev[i] = (e, counts[e])
                inc = (e, 1)
                v = dict(ck)
                v[e] = counts[e]
                vc[i] = v
            else:
                inc = None
            streams[e].append((waits, o["fn"], inc))
            o["waits"] = dict(waits)
            o["ev"] = ev[i]
        self.sem_counts = counts
        engobj = {"pe": "tensor", "dve": "vector", "act": "scalar", "pool": "gpsimd", "sp": "sync"}
        with nc.Block() as block:
            for e in self.ENGS:
                stream = streams[e]
                fin = outstanding if e == final_wait_eng else None

                def body(eng, stream=stream, fin=fin):
                    for waits, fn, inc in stream:
                        for s, v in waits.items():
                            eng.wait_ge(sems[s], v)
                        ins = fn(eng)
                        if inc is not None:
                            if isinstance(ins, (list, tuple)):
                                for x in ins:
                                    x.then_inc(sems[inc[0]], inc[1])
                            else:
                                ins.then_inc(sems[inc[0]], inc[1])
                    if fin:
                        for s, v in fin.items():
                            eng.wait_ge(sems[s], v)
                getattr(block, engobj[e])(body)


def _gammas():
    h = np.arange(NH, dtype=np.float64)
    return 1.0 - np.exp2(-5.0 - h)


def make_consts():
    g = _gammas()
    idx = np.arange(128, dtype=np.float64)
    c = {}
    c["c_ident"] = np.eye(128, dtype=np.float32)
    c["c_ones"] = np.ones((128, 128), dtype=np.float32)
    half = 64
    inv = (10000.0 ** (-(np.arange(half, dtype=np.float32) / np.float32(half)))).astype(np.float32)
    c["c_invf"] = np.ascontiguousarray(np.broadcast_to(inv[None, :], (128, half))).astype(np.float32)
    causal = (idx[None, :] >= idx[:, None]).astype(np.float64)
    mask = causal[:, None, :] * (g ** -128.0)[None, :, None]
    c["c_mask"] = mask.astype(np.float32)
    xi = g[:, None] ** (idx[None, :] + 1.0)
    c["c_xi"] = np.ascontiguousarray(np.broadcast_to(xi[None], (128, NH, 128))).astype(np.float32)
    zs = (g[None, :] ** (127.0 - idx[:, None])) * (128.0 ** -0.5)
    c["c_zs"] = zs.astype(np.float32)
    return c, [float(x) for x in (g ** 128.0)]


def build_program(nseq, seqlen, layers=(0, 1)):
    ngs = seqlen // T
    ng = nseq * ngs
    ntok = nseq * seqlen
    ncht = ntok // 128
    consts, cdec = make_consts()
    nc = bass.Bass("TRN2", target_bir_lowering=False)

    def din(name, shape, dt=F32):
        return nc.dram_tensor(name, list(shape), dt, kind="ExternalInput").ap()
    x_d = din("x", [ntok, D])
    mem_d = din("mem", [nseq * MEM, D])
    pos_d = din("pos", [128, ncht], I32)
    wkv_d = din("w_mem_kv", [D, D])
    w_in_d = [din("ret_w_in", [D, RET_IN]), din("conv_w_in", [D, CONV_IN])]
    w_out_d = [din("ret_w_out", [2048, D]), din("conv_w_out", [2048, D])]
    gmem_d = din("g_mem", [128, 8])
    gpre_d = din("g_pre", [128, 2, 8])
    gpost_d = din("g_post", [128, 2, D])
    dw_d = din("dw_w", [128, 12, CW])
    dwb_d = din("dw_b", [128, 12])
    lng_d = din("ln_g", [128, 12])
    lnb_d = din("ln_b", [128, 12])
    cd = {k: din(k, v.shape) for k, v in consts.items()}
    out_d = nc.dram_tensor("out", [ntok, D], F32, kind="ExternalOutput").ap()

    wkv_s = nc.dram_tensor("wkv_s", [2, 128, 8, 512], BF16).ap()
    win_s = [nc.dram_tensor("win0_s", [12, 128, 8, 512], BF16).ap(),
             nc.dram_tensor("win1_s", [11, 128, 8, 512], BF16).ap()]
    wout_s = [nc.dram_tensor("wout0_s", [2, 128, 16, 512], BF16).ap(),
              nc.dram_tensor("wout1_s", [2, 128, 16, 512], BF16).ap()]
    rope_s = nc.dram_tensor("rope_s", [3, 128, ncht, 64], F32).ap()

    with ExitStack() as st:
        P = Prog(nc, st)
        X = P.sb("X", [128, NCH, D], F32)
        hb = [P.sb(f"hb{i}", [128, D], BF16) for i in range(2)]
        junk = P.sb("junk", [128, D], BF16)
        hT = P.sb("hT", [128, 8, T], BF16)
        WS = [P.sb(f"WS{i}", [128, 8, 512], BF16) for i in range(3)]
        ARENA_B = 56 * 1024
        arena = P.sb("arena", [128, ARENA_B // 2], BF16)
        arena32 = arena.bitcast(F32)

        def a16(off_bytes, shape):
            n = int(np.prod(shape))
            ap = arena[:, off_bytes // 2: off_bytes // 2 + n]
            return ap

        def a32(off_bytes, shape):
            n = int(np.prod(shape))
            return arena32[:, off_bytes // 4: off_bytes // 4 + n]
        KB = 1024
        Qb = a16(0, [NCH * D]).rearrange("p (c f) -> p c f", c=NCH)
        Kb = a16(8 * KB, [NCH * D]).rearrange("p (c f) -> p c f", c=NCH)
        Vb = a16(16 * KB, [NCH * BW]).rearrange("p (c f) -> p c f", c=NCH)
        qT = [a16(28 * KB + i * 2 * KB, [NH * 128]).rearrange("p (h t) -> p h t", h=NH) for i in range(2)]
        kT = [a16(32 * KB + i * 2 * KB, [NH * 128]).rearrange("p (h t) -> p h t", h=NH) for i in range(2)]
        STb = [a16(36 * KB + i * 1 * KB, [4 * 128]).rearrange("p (h t) -> p h t", h=4) for i in range(4)]
        ONb = [a16(40 * KB + i * 3 * KB, [BW]) for i in range(2)]
        t1b = [a32(46 * KB + i * 2 * KB, [512]) for i in range(2)]
        t2b = [a32(50 * KB + i * 2 * KB, [512]) for i in range(2)]
        Wo = [a16(i * 16 * KB, [16 * 512]).rearrange("p (k n) -> p k n", k=16) for i in range(2)]
        otmp = a32(32 * KB, [D])
        convT = a32(0, [12 * T]).rearrange("p (c t) -> p c t", c=12)
        sigb = [a16(24 * KB + i * KB, [T]) for i in range(2)]
        ybb = [a16(26 * KB + i * KB, [T]) for i in range(2)]
        ysqb = [a16(28 * KB + i * KB, [T]) for i in range(2)]
        MEAN = a32(30 * KB, [T])
        RSTD = a32(32 * KB, [T])
        NMR = a32(34 * KB, [T])
        VAR = a32(36 * KB, [T])
        zb = [a32(38 * KB + i * 2 * KB, [T]) for i in range(2)]
        ab = [a16(42 * KB + i * KB, [T]) for i in range(2)]
        diagA = a16(44 * KB, [16 * 128]).rearrange("p (j c) -> p j c", j=16)
        diagB2 = [a16((48 + 4 * i) * KB, [16 * 128]).rearrange("p (j c) -> p j c", j=16) for i in range(2)]
        ARENA_KEYS = []

        xqT = P.sb("xqT", [128, 4, T], BF16)
        yT = P.sb("yT", [128, 16, T], BF16)
        pT = [P.sb(f"pT{i}", [128, T], BF16) for i in range(4)]
        rinv = P.sb("rinv", [128, T], F32)
        ontT = P.sb("ontT", [128, 12, 128], BF16)
        xtmp = P.sb("xtmp", [128, T], F32)
        rinv2 = [rinv, P.sb("rinv_b", [128, T], F32)]
        xtmp2 = [xtmp, P.sb("xtmp_b", [128, T], F32)]
        state = P.sb("state", [128, NH, DV], F32)
        state_bf = P.sb("state_bf", [128, NH, DV], BF16)
        gluT = P.sb("gluT", [128, 12, T + 32], BF16)
        ident = P.sb("ident", [128, 128], BF16)
        ones = P.sb("ones", [128, 128], BF16)
        identf = P.sb("identf", [128, 128], F32)
        invf = P.sb("invf", [128, 64], F32)
        mask = P.sb("mask", [128, NH, 128], F32)
        xi = P.sb("xi", [128, NH, 128], F32)
        zs = P.sb("zs", [128, NH], F32)
        gmem = P.sb("gmem", [128, 8], F32)
        gpre = P.sb("gpre", [128, 2, 8], F32)
        gpost = P.sb("gpost", [128, 2, D], F32)
        dw = P.sb("dw", [128, 12, CW], F32)
        dwb = P.sb("dwb", [128, 12], F32)
        lng = P.sb("lng", [128, 12], F32)
        lnb = P.sb("lnb", [128, 12], F32)
        mhalf = P.sb("mhalf", [128, T], F32)
        rope = P.sb("rope", [128, 3, NCH, 64], F32)
        posi = P.sb("posi", [128, ncht], I32)
        posf = P.sb("posf", [128, ncht], F32)
        rtmp = [P.sb(f"rtmp{i}", [128, NCH, 64], F32) for i in range(3)]
        memkT = P.sb("memkT", [128, 4, MEM], BF16)
        memv = P.sb("memv", [128, 2, 512], BF16)
        small = P.sb("small", [128, 512], F32)
        fdummy = P.sb("fdummy", [128, 1], F32)
        psum = P.ps("psum", [128, 8, 512], F32)
        psum16 = psum.bitcast(BF16)

        def bank(i):
            return psum[:, i, :]

        def bank16(i):
            return psum16[:, i, :]

        cnt = {"stat": 0}

        def stat(n):
            o = cnt["stat"]
            cnt["stat"] = (o + 1) % 128
            return small[:, 4 * o:4 * o + n], f"small{o}"

        def ld(q, sem, dst, src, key):
            P.dma(q, sem, lambda e: e.dma_start(out=dst, in_=src), writes=[key])
        ld("pool", "c0", ident[:], cd["c_ident"], "ident")
        ld("pool", "c1", ones[:], cd["c_ones"], "ones")
        ld("sp", "c1f", identf[:], cd["c_ident"], "identf")
        ld("sp", "c2", invf[:], cd["c_invf"], "invf")
        ld("sp", "c3", mask[:], cd["c_mask"], "mask")
        ld("sp", "c4", xi[:], cd["c_xi"], "xi")
        ld("sp", "c5", zs[:], cd["c_zs"], "zs")
        ld("sp", "c6", gmem[:], gmem_d, "gmem")
        ld("sp", "c7", gpre[:], gpre_d, "gpre")
        ld("sp", "c8", gpost[:], gpost_d, "gpost")
        ld("sp", "c9", dw[:], dw_d, "dw")
        ld("sp", "c10", dwb[:], dwb_d, "dwb")
        ld("sp", "c11", lng[:], lng_d, "lng")
        ld("sp", "c12", lnb[:], lnb_d, "lnb")
        ld("sp", "c13", posi[:], pos_d, "posi")
        P.pool(lambda e: e.memset(mhalf[:], -0.5), writes=["mhalf"])

        def cast_in(sem, dst, src_w, c0, key):
            P.dma("pool", sem, lambda e: e.dma_start(
                out=dst, in_=src_w.rearrange("(k p) n -> p k n", p=128)[:, :, c0:c0 + 512]), writes=[key])

        def cast_out(sem, dst, src_w, c0, key):
            P.dma("pool", sem, lambda e: e.dma_start(
                out=dst, in_=src_w.rearrange("(k p) n -> p k n", p=128)[:, :, c0:c0 + 512]), writes=[key])
        for b in range(2):
            cast_in(f"ck{b}", wkv_s[b], wkv_d, b * 512, f"wkv_s{b}")
        L0_ORDER = list(range(12))
        L1_ORDER = [0, 3, 1, 4, 2, 5, 6, 7, 8, 9, 10]
        if 0 in layers:
            for b in L0_ORDER:
                cast_in(f"cw0_{b}", win_s[0][b], w_in_d[0], b * 512, f"win0_{b}")
            for b in range(2):
                cast_out(f"co0_{b}", wout_s[0][b], w_out_d[0], b * 512, f"wout0_{b}")
        if 1 in layers:
            for b in L1_ORDER:
                cast_in(f"cw1_{b}", win_s[1][b], w_in_d[1], b * 512, f"win1_{b}")
            for b in range(2):
                cast_out(f"co1_{b}", wout_s[1][b], w_out_d[1], b * 512, f"wout1_{b}")

        wlist = []
        for s in range(nseq):
            wlist += [(wkv_s[0], "wkv_s0"), (wkv_s[1], "wkv_s1")]
            for g in range(ngs):
                if 0 in layers:
                    wlist += [(win_s[0][b], f"win0_{b}") for b in L0_ORDER]
                if 1 in layers:
                    wlist += [(win_s[1][b], f"win1_{b}") for b in L1_ORDER]
        wstate = {"next_load": 0, "next_use": 0}

        def w_issue():
            i = wstate["next_load"]
            if i >= len(wlist):
                return
            src, key = wlist[i]
            sl = i % 3
            P.dma("sp", f"ws{sl}", lambda e: e.dma_start(out=WS[sl][:], in_=src), reads=[key], writes=[f"WS{sl}"])
            wstate["next_load"] = i + 1

        def w_next():
            i = wstate["next_use"]
            wstate["next_use"] = i + 1
            return WS[i % 3], f"WS{i % 3}"
        for _ in range(3):
            w_issue()

        def fence():
            P.pool(lambda e: e.memset(fdummy[:], 0.0), writes=["ar_all", "fdummy"])

        P.dve(lambda e: e.tensor_copy(out=posf[:], in_=posi[:]), reads=["posi"], writes=["posf"])
        for g in range(ng):
            ang, red, r = rtmp
            P.dve(lambda e, g=g: e.tensor_tensor(
                out=ang[:], in0=posf[:, g * NCH:(g + 1) * NCH].unsqueeze(2).broadcast_to([128, NCH, 64]),
                in1=invf[:].unsqueeze(1).broadcast_to([128, NCH, 64]), op=ALU.mult),
                reads=["posf", "invf"], writes=["rt0"])
            for which, shift in ((1, 0.0), (0, math.pi / 2)):
                def f_red(e, shift=shift):
                    e.tensor_scalar(out=red[:], in0=ang[:], scalar1=shift, scalar2=1.0 / TWO_PI,
                                    op0=ALU.add, op1=ALU.mult)
                    e.tensor_scalar(out=red[:], in0=red[:], scalar1=MAGIC, scalar2=None, op0=ALU.add)
                    return e.tensor_scalar(out=red[:], in0=red[:], scalar1=-MAGIC, scalar2=-TWO_PI,
                                           op0=ALU.add, op1=ALU.mult)
                P.dve(lambda e, shift=shift: e.tensor_scalar(
                    out=red[:], in0=ang[:], scalar1=shift, scalar2=1.0 / TWO_PI, op0=ALU.add, op1=ALU.mult),
                    reads=["rt0"], writes=["rt1"])
                P.dve(lambda e: e.tensor_scalar(out=r[:], in0=red[:], scalar1=MAGIC, scalar2=None, op0=ALU.add),
                      reads=["rt1"], writes=["rt2"])
                P.dve(lambda e: e.tensor_scalar(out=red[:], in0=r[:], scalar1=-MAGIC, scalar2=-TWO_PI,
                                                op0=ALU.add, op1=ALU.mult),
                      reads=["rt2"], writes=["rt1"])
                P.dve(lambda e, shift=shift: e.scalar_tensor_tensor(
                    out=r[:], in0=ang[:], scalar=shift, in1=red[:], op0=ALU.add, op1=ALU.add),
                    reads=["rt0", "rt1"], writes=["rt2"])
                P.dve(lambda e: e.tensor_scalar(out=red[:], in0=r[:], scalar1=3.14159, scalar2=-3.14159,
                                                op0=ALU.min, op1=ALU.max),
                      reads=["rt2"], writes=["rt1"])
                P.act(lambda e, which=which: e.activation(out=rope[:, which], in_=red[:], func=AF.Sin),
                      reads=["rt1"], writes=[f"rope{which}"])
            P.act(lambda e: e.mul(out=rope[:, 2], in_=rope[:, 1], mul=-1.0), reads=["rope1"], writes=["rope2"])
            P.dma("sp", "ropest", lambda e, g=g: e.dma_start(
                out=rope_s[:, :, g * NCH:(g + 1) * NCH, :].rearrange("w p c d -> p w c d"), in_=rope[:]),
                reads=["rope0", "rope1", "rope2"], writes=[f"rope_s{g}"])

        def norm_transpose(src_ap, src_key, gsc_ap, gsc_key, dst_col0, tr_bank, hbi):
            ss, kss = stat(1)
            rs, krs = stat(1)
            hbt = hb[hbi]
            P.act(lambda e: e.activation(out=junk[:], in_=src_ap, func=AF.Square, accum_out=ss),
                  reads=[src_key], writes=["junk", kss])
            P.dve(lambda e: e.tensor_scalar(out=rs, in0=ss, scalar1=1.0 / D, scalar2=EPS, op0=ALU.mult, op1=ALU.add),
                  reads=[kss], writes=[krs])
            P.pool(lambda e: e.tensor_tensor(out=rs, in0=rs, in1=mhalf[:, 0:1], op=ALU.pow),
                   reads=[krs, "mhalf"], writes=[krs])
            P.act(lambda e: e.activation(out=hbt[:], in_=src_ap, func=AF.Copy, scale=rs),
                  reads=[src_key, krs], writes=[f"hb{hbi}"])
            pb = bank16(tr_bank)

            def ftr(e):
                ins = None
                for k in range(8):
                    ins = e.transpose(out=pb[:, k * 128:(k + 1) * 128], in_=hbt[:, k * 128:(k + 1) * 128],
                                      identity=ident[:])
                return ins
            P.pe(ftr, reads=[f"hb{hbi}", "ident"], writes=[f"ps{tr_bank}"])
            P.dve(lambda e: e.tensor_tensor(
                out=hT[:, :, dst_col0:dst_col0 + 128], in0=pb.rearrange("p (k t) -> p k t", k=8),
                in1=gsc_ap.unsqueeze(2).broadcast_to([128, 8, 128]), op=ALU.mult),
                reads=[f"ps{tr_bank}", gsc_key], writes=[f"hT{dst_col0 // 128}"])

        def norm_transpose_group(L):
            ss4, kss = stat(4)
            rs4, krs = stat(4)
            for c in range(NCH):
                P.act(lambda e, c=c: e.activation(out=junk[:], in_=X[:, c, :], func=AF.Square,
                                                  accum_out=ss4[:, c:c + 1]),
                      reads=[f"X{c}"], writes=["junk", kss + f"_{c}"])
            P.dve(lambda e: e.tensor_scalar(out=rs4, in0=ss4, scalar1=1.0 / D, scalar2=EPS, op0=ALU.mult, op1=ALU.add),
                  reads=[kss + f"_{c}" for c in range(NCH)], writes=[krs])
            P.pool(lambda e: e.tensor_tensor(out=rs4, in0=rs4, in1=mhalf[:, 0:4], op=ALU.pow),
                   reads=[krs, "mhalf"], writes=[krs])
            for c in range(NCH):
                hbi = c % 2
                hbt = hb[hbi]
                tr_bank = 2 + (c % 2)
                P.act(lambda e, c=c, hbt=hbt: e.activation(out=hbt[:], in_=X[:, c, :], func=AF.Copy,
                                                         scale=rs4[:, c:c + 1]),
                      reads=[f"X{c}", krs], writes=[f"hb{hbi}"])
                pb = bank16(tr_bank)

                def ftr(e, hbt=hbt, pb=pb):
                    ins = None
                    for k in range(8):
                        ins = e.transpose(out=pb[:, k * 128:(k + 1) * 128], in_=hbt[:, k * 128:(k + 1) * 128],
                                          identity=ident[:])
                    return ins
                P.pe(ftr, reads=[f"hb{hbi}", "ident"], writes=[f"ps{tr_bank}"])
                P.dve(lambda e, c=c, pb=pb: e.tensor_tensor(
                    out=hT[:, :, c * 128:(c + 1) * 128], in0=pb.rearrange("p (k t) -> p k t", k=8),
                    in1=gpre[:, L, :].unsqueeze(2).broadcast_to([128, 8, 128]), op=ALU.mult),
                    reads=[f"ps{tr_bank}", "gpre"], writes=[f"hT{c}"])

        def mm_group(out_ap, pairs, reads, wkey):
            def f(e):
                ins = None
                n = len(pairs)
                for i, (l, r) in enumerate(pairs):
                    ins = e.matmul(out_ap, lhsT=l, rhs=r, start=(i == 0), stop=(i == n - 1))
                return ins
            P.pe(f, reads=reads, writes=[wkey])

        hT_keys = [f"hT{c}" for c in range(NCH)]

        def mem_kv(s):
            for mc in range(2):
                P.dma("sp", f"xl{mc}", lambda e, mc=mc: e.dma_start(
                    out=X[:, mc, :], in_=mem_d[s * MEM + mc * 128: s * MEM + (mc + 1) * 128, :]),
                    writes=[f"X{mc}"])
            for mc in range(2):
                norm_transpose(X[:, mc, :], f"X{mc}", gmem[:], "gmem", mc * 128, mc, mc)
            w0, k0 = w_next()
            for h in range(4):
                bk = 2 + (h % 2)
                mm_group(bank(bk)[:, 0:MEM],
                         [(w0[:, k, h * 128:(h + 1) * 128], hT[:, k, 0:MEM]) for k in range(8)],
                         [k0, "hT0", "hT1"], f"ps{bk}")
                P.dve(lambda e, h=h, bk=bk: e.tensor_copy(out=memkT[:, h, :], in_=bank(bk)[:, 0:MEM]),
                      reads=[f"ps{bk}"], writes=["memkT"])
            w_issue()
            w1, k1 = w_next()
            for mc in range(2):
                bk = 4 + mc
                mm_group(bank(bk), [(hT[:, k, mc * 128:(mc + 1) * 128], w1[:, k, :]) for k in range(8)],
                         [k1, f"hT{mc}"], f"ps{bk}")
                P.act(lambda e, mc=mc, bk=bk: e.activation(out=memv[:, mc, :], in_=bank(bk), func=AF.Copy),
                      reads=[f"ps{bk}"], writes=["memv"])
            w_issue()

        def mem_attn():
            sc = 128.0 ** -0.5
            for hd in range(4):
                par = hd % 2
                b0 = 4 * par
                pts = [pT[2 * par + mc] for mc in range(2)]
                ptk = [f"pT{2 * par + mc}" for mc in range(2)]
                ri, xt = rinv2[par], xtmp2[par]
                for mc in range(2):
                    bk = b0 + mc
                    mm_group(bank(bk), [(memkT[:, hd, mc * 128:(mc + 1) * 128], xqT[:, hd, :])],
                             ["memkT", "xqT"], f"ps{bk}")
                    P.act(lambda e, mc=mc, bk=bk, pts=pts: e.activation(out=pts[mc][:], in_=bank(bk), func=AF.Exp, scale=sc),
                          reads=[f"ps{bk}"], writes=[ptk[mc]])
                mm_group(bank(b0 + 2), [(memv[:, mc, hd * 128:(hd + 1) * 128], pts[mc][:]) for mc in range(2)],
                         ["memv"] + ptk, f"ps{b0 + 2}")
                mm_group(bank(b0 + 3), [(ones[:], pts[mc][:]) for mc in range(2)], ["ones"] + ptk, f"ps{b0 + 3}")
                P.dve(lambda e, ri=ri, b0=b0: e.reciprocal(out=ri[:], in_=bank(b0 + 3)), reads=[f"ps{b0 + 3}"],
                      writes=[f"rinv{par}"])
                P.dve(lambda e, ri=ri, xt=xt, b0=b0: e.tensor_tensor(out=xt[:], in0=bank(b0 + 2), in1=ri[:], op=ALU.mult),
                      reads=[f"ps{b0 + 2}", f"rinv{par}"], writes=[f"xtmp{par}"])
                P.dve(lambda e, hd=hd, xt=xt: e.tensor_tensor(out=yT[:, 12 + hd, :], in0=xt[:], in1=yT[:, 12 + hd, :],
                                                              op=ALU.mult),
                      reads=[f"xtmp{par}", f"yT{12 + hd}"], writes=[f"yT{12 + hd}"])

        def wo_prefetch(L):
            if L == 0:
                dead = [[f"Q{c}" for c in range(NCH)] + [f"K{c}" for c in range(NCH)],
                        [f"V{c}" for c in range(NCH)] + ["qT0", "qT1"]]
            else:
                dead = [[f"conv{cb}" for cb in range(8)],
                        [f"conv{cb}" for cb in range(8, 12)] + ["sig0", "sig1", "yb0", "yb1", "ysq0", "ysq1", "MEAN"]]
            for nb in range(2):
                P.dma("sp", f"wo{nb}", lambda e, nb=nb: e.dma_start(out=Wo[nb], in_=wout_s[L][nb]),
                      reads=[f"wout{L}_{nb}"], writes=[f"Wo{nb}"] + dead[nb])

        def out_proj(L, tok0, last_layer):
            fence()
            ykeys = [f"yT{m}" for m in range(16)]
            for c in range(NCH):
                b0 = 4 + 2 * (c % 2)
                for nb in range(2):
                    mm_group(bank(b0 + nb), [(yT[:, m, c * 128:(c + 1) * 128], Wo[nb][:, m, :]) for m in range(16)],
                             ykeys + [f"Wo{nb}"] + ARENA_KEYS, f"ps{b0 + nb}")
                ss2, kss2 = stat(2)
                rs, krs = stat(1)
                for nb in range(2):
                    P.act(lambda e, nb=nb, ss2=ss2, b0=b0: e.activation(out=junk[:, 0:512], in_=bank(b0 + nb), func=AF.Square,
                                                                 accum_out=ss2[:, nb:nb + 1]),
                          reads=[f"ps{b0 + nb}"], writes=["junk", kss2 + f"_{nb}"])
                P.dve(lambda e, ss2=ss2, rs=rs: e.tensor_tensor(out=rs, in0=ss2[:, 0:1], in1=ss2[:, 1:2], op=ALU.add),
                      reads=[kss2 + "_0", kss2 + "_1"], writes=[krs])
                P.dve(lambda e, rs=rs: e.tensor_scalar(out=rs, in0=rs, scalar1=1.0 / D, scalar2=EPS,
                                                      op0=ALU.mult, op1=ALU.add),
                      reads=[krs], writes=[krs])
                P.pool(lambda e, rs=rs: e.tensor_tensor(out=rs, in0=rs, in1=mhalf[:, 0:1], op=ALU.pow),
                       reads=[krs, "mhalf"], writes=[krs])
                P.dve(lambda e, b0=b0: e.tensor_tensor(
                    out=otmp, in0=psum[:, b0:b0 + 2, :].rearrange("p a b -> p (a b)"), in1=gpost[:, L, :], op=ALU.mult),
                    reads=[f"ps{b0}", f"ps{b0 + 1}", "gpost"] + ARENA_KEYS, writes=["otmp"])
                P.dve(lambda e, c=c, rs=rs: e.scalar_tensor_tensor(
                    out=X[:, c, :], in0=otmp, scalar=rs, in1=X[:, c, :], op0=ALU.mult, op1=ALU.add),
                    reads=["otmp", krs, f"X{c}"], writes=[f"X{c}"])
                if last_layer:
                    P.dma("sp", f"xs{c}", lambda e, c=c: e.dma_start(
                        out=out_d[tok0 + c * 128: tok0 + (c + 1) * 128, :], in_=X[:, c, :]),
                        reads=[f"X{c}"])
            fence()

        IPB = [0, 1, 4, 5, 6, 7]

        def feature_major_blocks(nblk, first_mc, silu):
            for b in range(nblk):
                w, wk = w_next()
                for s in range(4):
                    bk = IPB[(b * 4 + s) % len(IPB)]
                    mm_group(bank(bk), [(w[:, k, s * 128:(s + 1) * 128], hT[:, k, :]) for k in range(8)],
                             [wk] + hT_keys, f"ps{bk}")
                    mc = first_mc + 4 * b + s
                    if silu:
                        P.act(lambda e, mc=mc, bk=bk: e.activation(out=yT[:, mc, :], in_=bank(bk), func=AF.Silu),
                              reads=[f"ps{bk}"], writes=[f"yT{mc}"])
                    else:
                        P.act(lambda e, mc=mc, bk=bk: e.activation(out=xqT[:, mc, :], in_=bank(bk), func=AF.Copy),
                              reads=[f"ps{bk}"], writes=["xqT"])
                w_issue()

        def layer0(gi, gs, tok0):
            if DBG_STAGE < 1:
                return
            norm_transpose_group(0)
            if DBG_STAGE < 2:
                return
            cosb = rope[:, 0]
            sinb = rope[:, 1]
            nsinb = rope[:, 2]
            for blk in range(4):
                w, wk = w_next()
                dst = Qb if blk < 2 else Kb
                dkey = "Q" if blk < 2 else "K"
                col0 = (blk % 2) * 512
                for c in range(NCH):
                    bk = IPB[(blk * NCH + c) % len(IPB)]
                    mm_group(bank(bk), [(hT[:, k, c * 128:(c + 1) * 128], w[:, k, :]) for k in range(8)],
                             [wk, f"hT{c}"] + ARENA_KEYS, f"ps{bk}")
                    t1 = t1b[c % 2]
                    t2 = t2b[c % 2]
                    p4 = bank(bk).rearrange("p (h two d) -> p h two d", h=4, two=2)
                    t14 = t1.rearrange("p (h two d) -> p h two d", h=4, two=2)
                    t24 = t2.rearrange("p (h two d) -> p h two d", h=4, two=2)
                    P.dve(lambda e, c=c, p4=p4, t14=t14: e.tensor_tensor(
                        out=t14, in0=p4, in1=cosb[:, c, :].unsqueeze(1).unsqueeze(1).broadcast_to([128, 4, 2, 64]),
                        op=ALU.mult), reads=[f"ps{bk}", "rope"] + ARENA_KEYS, writes=[f"t1_{c % 2}"])

                    def frot(e, c=c, p4=p4, t24=t24):
                        e.tensor_tensor(out=t24[:, :, 0, :], in0=p4[:, :, 1, :],
                                        in1=nsinb[:, c, :].unsqueeze(1).broadcast_to([128, 4, 64]), op=ALU.mult)
                        return e.tensor_tensor(out=t24[:, :, 1, :], in0=p4[:, :, 0, :],
                                               in1=sinb[:, c, :].unsqueeze(1).broadcast_to([128, 4, 64]), op=ALU.mult)
                    P.dve(frot, reads=[f"ps{bk}", "rope"] + ARENA_KEYS, writes=[f"t2_{c % 2}"])
                    P.pool(lambda e, c=c, t1=t1, t2=t2, dst=dst, col0=col0: e.tensor_tensor(
                        out=dst[:, c, col0:col0 + 512], in0=t1, in1=t2, op=ALU.add),
                        reads=[f"t1_{c % 2}", f"t2_{c % 2}"] + ARENA_KEYS, writes=[f"{dkey}{c}"])
                w_issue()
            if DBG_STAGE < 3:
                return
            for vb in range(3):
                w, wk = w_next()
                for c in range(NCH):
                    bk = IPB[(vb * NCH + c) % len(IPB)]
                    mm_group(bank(bk), [(hT[:, k, c * 128:(c + 1) * 128], w[:, k, :]) for k in range(8)],
                             [wk, f"hT{c}"], f"ps{bk}")

                    def fv(e, c=c, bk=bk, vb=vb):
                        ins = None
                        for h in range(NH):
                            lo = max(DV * h, 512 * vb)
                            hi = min(DV * h + DV, 512 * vb + 512)
                            if lo >= hi:
                                continue
                            ins = e.activation(out=Vb[:, c, lo:hi], in_=bank(bk)[:, lo - 512 * vb: hi - 512 * vb],
                                               func=AF.Copy, scale=zs[:, h:h + 1])
                        return ins
                    P.act(fv, reads=[f"ps{bk}", "zs"] + ARENA_KEYS, writes=[f"V{c}"])
                w_issue()
            if DBG_STAGE < 4:
                return
            feature_major_blocks(1, 0, False)
            feature_major_blocks(4, 0, True)
            if DBG_STAGE < 5:
                return
            if gs == 0:
                P.dve(lambda e: e.memset(state[:], 0.0), writes=["state0", "state1"])
                P.pool(lambda e: e.memset(state_bf[:], 0.0), writes=["stbf0", "stbf1"])
            def head(c):
                bi = c % 2
                for (src, skey, dstT, dkey, bk, scaled) in ((Qb, "Q", qT[bi], f"qT{bi}", 0, True),
                                                            (Kb, "K", kT[bi], f"kT{bi}", 1, False)):
                    pb = bank16(bk)

                    def ftr(e, src=src, pb=pb, c=c):
                        ins = None
                        for h in range(NH):
                            ins = e.transpose(out=pb[:, h * 128:(h + 1) * 128], in_=src[:, c, h * 128:(h + 1) * 128],
                                              identity=ident[:])
                        return ins
                    P.pe(ftr, reads=[f"{skey}{c}", "ident"] + ARENA_KEYS, writes=[f"ps{bk}"])
                    if scaled:
                        P.dve(lambda e, pb=pb, dstT=dstT: e.tensor_tensor(
                            out=dstT, in0=pb.rearrange("p (h t) -> p h t", h=NH), in1=xi[:], op=ALU.mult),
                            reads=[f"ps{bk}", "xi"] + ARENA_KEYS, writes=[dkey])
                    else:
                        P.act(lambda e, pb=pb, dstT=dstT: e.activation(
                            out=dstT, in_=pb.rearrange("p (h t) -> p h t", h=NH), func=AF.Copy),
                            reads=[f"ps{bk}"] + ARENA_KEYS, writes=[dkey])
            def halves(c, hh_list, phase="AB"):
                bi = c % 2
                for hh in hh_list:
                    hs = list(range(4 * hh, 4 * hh + 4))
                    ST = STb[2 * bi + hh]
                    skey = f"ST{2 * bi + hh}"
                    ob = 3 if hh == 0 else 5
                    okeys = [f"ps{ob}", f"ps{ob + 1}"]
                    if "A" in phase:
                        def fs(e, hs=hs, bi=bi):
                            ins = None
                            for n, h in enumerate(hs):
                                ins = e.matmul(bank(2)[:, n * 128:(n + 1) * 128], lhsT=kT[bi][:, h, :], rhs=qT[bi][:, h, :],
                                               start=True, stop=True)
                            return ins
                        P.pe(fs, reads=[f"qT{bi}", f"kT{bi}"] + ARENA_KEYS, writes=["ps2"])
                        P.dve(lambda e, ST=ST, hh=hh: e.tensor_tensor(
                            out=ST, in0=bank(2).rearrange("p (h t) -> p h t", h=4), in1=mask[:, 4 * hh:4 * hh + 4, :],
                            op=ALU.mult), reads=["ps2", "mask"] + ARENA_KEYS, writes=[skey])
                        if DBG_STAGE < 4.2:
                            continue

                        def fo(e, hs=hs, bi=bi, ST=ST, c=c, ob=ob):
                            ins = None
                            for n, h in enumerate(hs):
                                o = bank(ob + n // 2)[:, (n % 2) * DV:(n % 2) * DV + DV]
                                e.matmul(o, lhsT=ST[:, n, :], rhs=Vb[:, c, h * DV:(h + 1) * DV], start=True, stop=False)
                                ins = e.matmul(o, lhsT=qT[bi][:, h, :], rhs=state_bf[:, h, :], start=False, stop=True)
                            return ins
                        P.pe(fo, reads=[skey, f"V{c}", f"qT{bi}", f"stbf{hh}"] + ARENA_KEYS, writes=okeys)
                        if DBG_STAGE < 4.3:
                            continue
                        last_chunk = False
                        if not last_chunk:
                            for pp in range(2):
                                def fd(e, hs=hs, c=c, pp=pp):
                                    ins = None
                                    for n in (2 * pp, 2 * pp + 1):
                                        h = hs[n]
                                        o = bank(7)[:, (n % 2) * DV:(n % 2) * DV + DV]
                                        ins = e.matmul(o, lhsT=Kb[:, c, h * 128:(h + 1) * 128],
                                                       rhs=Vb[:, c, h * DV:(h + 1) * DV], start=True, stop=True)
                                    return ins
                                P.pe(fd, reads=[f"K{c}", f"V{c}"] + ARENA_KEYS, writes=["ps7"])

                                def fst(e, hs=hs, pp=pp):
                                    ins = None
                                    for n in (2 * pp, 2 * pp + 1):
                                        h = hs[n]
                                        o = bank(7)[:, (n % 2) * DV:(n % 2) * DV + DV]
                                        ins = e.scalar_tensor_tensor(out=state[:, h, :], in0=state[:, h, :], scalar=cdec[h],
                                                                     in1=o, op0=ALU.mult, op1=ALU.add)
                                    return ins
                                P.dve(fst, reads=["ps7", f"state{hh}"], writes=[f"state{hh}"])
                            P.act(lambda e, hh=hh: e.activation(out=state_bf[:, 4 * hh:4 * hh + 4, :],
                                                               in_=state[:, 4 * hh:4 * hh + 4, :], func=AF.Copy),
                                  reads=[f"state{hh}"], writes=[f"stbf{hh}"])
                    if "B" in phase:
                        if DBG_STAGE < 4.4 or "stats" in SKIP:
                            continue
                        o4 = psum[:, ob:ob + 2, 0:2 * DV].rearrange("p b (h v) -> p b h v", h=2)
                        sm, ksm = stat(4)
                        sq, ksq = stat(4)
                        mu, kmu = stat(4)
                        rs, krs = stat(4)
                        nm, knm = stat(4)
                        sqs = t1b[0].rearrange("p (b h v) -> p b h v", b=2, h=2)[:, :, :, 0:DV] if False else None
                        P.dve(lambda e, sm=sm, o4=o4: e.tensor_reduce(
                            out=sm.rearrange("p (b h) -> p b h", b=2), in_=o4, axis=AX.X, op=ALU.add),
                            reads=okeys, writes=[ksm])

                        def fsq(e, sq=sq, ob=ob):
                            ins = None
                            for n in range(4):
                                o = bank(ob + n // 2)[:, (n % 2) * DV:(n % 2) * DV + DV]
                                ins = e.activation(out=junk[:, n * DV:(n + 1) * DV], in_=o, func=AF.Square, accum_out=sq[:, n:n + 1])
                            return ins
                        P.act(fsq, reads=okeys, writes=["junk", ksq])
                        P.dve(lambda e, sm=sm, mu=mu: e.tensor_scalar(out=mu, in0=sm, scalar1=1.0 / DV, scalar2=None,
                                                                     op0=ALU.mult), reads=[ksm], writes=[kmu])
                        P.dve(lambda e, mu=mu, nm=nm: e.tensor_tensor(out=nm, in0=mu, in1=mu, op=ALU.mult),
                              reads=[kmu], writes=[knm])
                        P.dve(lambda e, sq=sq, nm=nm, rs=rs: e.scalar_tensor_tensor(
                            out=rs, in0=sq, scalar=1.0 / DV, in1=nm, op0=ALU.mult, op1=ALU.subtract),
                            reads=[ksq, knm], writes=[krs])
                        P.dve(lambda e, rs=rs: e.tensor_scalar(out=rs, in0=rs, scalar1=EPS, scalar2=None, op0=ALU.add),
                              reads=[krs], writes=[krs])
                        P.pool(lambda e, rs=rs: e.tensor_tensor(out=rs, in0=rs, in1=mhalf[:, 0:4],
                                                                op=ALU.pow), reads=[krs, "mhalf"], writes=[krs])
                        P.dve(lambda e, mu=mu, rs=rs, nm=nm: e.scalar_tensor_tensor(
                            out=nm, in0=mu, scalar=-1.0, in1=rs, op0=ALU.mult, op1=ALU.mult),
                            reads=[kmu, krs], writes=[knm])
                        if DBG_STAGE < 4.5 or "fap" in SKIP:
                            continue
                        ON = ONb[bi]

                        def fap(e, hs=hs, rs=rs, nm=nm, ON=ON, ob=ob):
                            ins = None
                            for n, h in enumerate(hs):
                                o = bank(ob + n // 2)[:, (n % 2) * DV:(n % 2) * DV + DV]
                                ins = e.activation(out=ON[:, h * DV:(h + 1) * DV], in_=o, func=AF.Identity,
                                                   scale=rs[:, n:n + 1], bias=nm[:, n:n + 1])
                            return ins
                        P.act(fap, reads=okeys + [krs, knm] + ARENA_KEYS, writes=[f"ON{bi}_{hh}"])

            def tail(c):
                bi = c % 2
                ON = ONb[bi]
                for p_ in range(2):
                    m0, m1 = 6 * p_, 6 * p_ + 6

                    def ftr2(e, ON=ON, m0=m0, m1=m1):
                        ins = None
                        for m in range(m0, m1):
                            ins = e.transpose(out=bank16(2)[:, (m - m0) * 128:(m - m0 + 1) * 128],
                                              in_=ON[:, m * 128:(m + 1) * 128], identity=ident[:])
                        return ins
                    P.pe(ftr2, reads=[f"ON{bi}_0", f"ON{bi}_1", "ident"], writes=["ps2"])
                    tmpb = ontT[:, m0:m1, :]
                    P.act(lambda e, tmpb=tmpb: e.activation(
                        out=tmpb, in_=bank16(2)[:, 0:6 * 128].rearrange("p (m t) -> p m t", m=6), func=AF.Copy),
                        reads=["ps2"], writes=[f"ontT{p_}"])
                    P.dve(lambda e, c=c, m0=m0, m1=m1, tmpb=tmpb: e.tensor_tensor(
                        out=yT[:, m0:m1, c * 128:(c + 1) * 128], in0=tmpb,
                        in1=yT[:, m0:m1, c * 128:(c + 1) * 128], op=ALU.mult),
                        reads=[f"ontT{p_}"] + [f"yT{m}" for m in range(m0, m1)],
                        writes=[f"yT{m}" for m in range(m0, m1)])

            head(0)
            halves(0, [0, 1], "A")
            halves(0, [0, 1], "B")
            for c in range(1, NCH):
                head(c)
                halves(c, [0, 1], "A")
                tail(c - 1)
                halves(c, [0, 1], "B")
            tail(NCH - 1)
            if "pad" in SKIP:
                for i in range(60):
                    if "padscale" in SKIP:
                        P.act(lambda e: e.activation(out=xtmp[:, 0:16], in_=rinv[:, 0:16], func=AF.Copy, scale=zs[:, 0:1]),
                              reads=["rinv", "zs"], writes=["xtmp"])
                    elif "padbias" in SKIP:
                        P.act(lambda e: e.activation(out=xtmp[:, 0:16], in_=rinv[:, 0:16], func=AF.Identity, bias=zs[:, 0:1]),
                              reads=["rinv", "zs"], writes=["xtmp"])
                    else:
                        P.act(lambda e: e.activation(out=xtmp[:, 0:16], in_=rinv[:, 0:16], func=AF.Copy),
                              reads=["rinv"], writes=["xtmp"])
            if DBG_STAGE < 6:
                return
            wo_prefetch(0)
            mem_attn()
            if DBG_STAGE < 7:
                return
            out_proj(0, tok0, last_layer=(1 not in layers))

        def layer1(gi, gs, tok0):
            norm_transpose_group(1)
            if gs == 0:
                P.pool(lambda e: e.memset(gluT[:, :, 0:30], 0.0), writes=["gluhist"])
                P.pool(lambda e: e.memset(gluT[:, :, T + 30:T + 32], 0.0), writes=["glupad"])
            for i in range(3):
                wu, ku = w_next()
                wg, kg = w_next()
                for s in range(4):
                    cb = 4 * i + s
                    ba = 2 * (s % 2)
                    mm_group(bank(ba), [(wu[:, k, s * 128:(s + 1) * 128], hT[:, k, :]) for k in range(8)],
                             [ku] + hT_keys, f"ps{ba}")
                    mm_group(bank(ba + 1), [(wg[:, k, s * 128:(s + 1) * 128], hT[:, k, :]) for k in range(8)],
                             [kg] + hT_keys, f"ps{ba + 1}")
                    sg = sigb[s % 2]
                    P.act(lambda e, sg=sg, ba=ba: e.activation(out=sg, in_=bank(ba + 1), func=AF.Sigmoid),
                          reads=[f"ps{ba + 1}"] + ARENA_KEYS, writes=[f"sig{s % 2}"])
                    P.dve(lambda e, sg=sg, ba=ba, cb=cb: e.tensor_tensor(
                        out=gluT[:, cb, 30:30 + T], in0=bank(ba), in1=sg, op=ALU.mult),
                        reads=[f"ps{ba}", f"sig{s % 2}"] + ARENA_KEYS, writes=[f"glu{cb}"])
                w_issue()
                w_issue()
            feature_major_blocks(1, 0, False)
            feature_major_blocks(4, 0, True)
            def build_b(cb):
                dB = diagB2[cb % 2]

                def fdb(e, cb=cb, dB=dB):
                    ins = None
                    for j in range(15):
                        ins = e.activation(out=dB[:, j, :], in_=ident[:], func=AF.Copy, scale=dw[:, cb, 16 + j:17 + j])
                    return ins
                P.act(fdb, reads=["ident", "dw"], writes=[f"diagB{cb % 2}"])

            build_b(0)
            for cb in range(12):
                bk = 6 + (cb % 2)
                dB = diagB2[cb % 2]

                def fda(e, cb=cb):
                    ins = None
                    for j in range(16):
                        ins = e.tensor_scalar(out=diagA[:, j, :], in0=identf[:], scalar1=dw[:, cb, j:j + 1], scalar2=None,
                                              op0=ALU.mult)
                    return ins
                P.dve(fda, reads=["identf", "dw"], writes=["diagA"])
                if cb + 1 < 12:
                    build_b(cb + 1)

                def fca(e, cb=cb, bk=bk):
                    ins = None
                    for j in range(16):
                        ins = e.matmul(bank(bk), lhsT=diagA[:, j, :], rhs=gluT[:, cb, j:j + T], start=(j == 0), stop=False)
                    return ins
                P.pe(fca, reads=["diagA", f"glu{cb}", "gluhist"], writes=[f"ps{bk}"])

                def fcb(e, cb=cb, bk=bk, dB=dB):
                    ins = None
                    for j in range(15):
                        k = 16 + j
                        ins = e.matmul(bank(bk), lhsT=dB[:, j, :], rhs=gluT[:, cb, k:k + T], start=False, stop=(j == 14))
                    return ins
                P.pe(fcb, reads=[f"diagB{cb % 2}", f"glu{cb}", "gluhist"], writes=[f"ps{bk}"])
                P.act(lambda e, cb=cb, bk=bk: e.activation(out=convT[:, cb, :], in_=bank(bk), func=AF.Identity,
                                                           bias=dwb[:, cb:cb + 1]),
                      reads=[f"ps{bk}", "dwb"], writes=[f"conv{cb}"])
            P.act(lambda e: e.activation(out=gluT[:, :, 0:30], in_=gluT[:, :, T:T + 30], func=AF.Copy),
                  reads=[f"glu{cb}" for cb in range(12)] + ["gluhist"], writes=["gluhist"])
            for cb in range(12):
                yb = ybb[cb % 2]
                ysq = ysqb[cb % 2]
                P.act(lambda e, cb=cb, yb=yb: e.activation(out=yb, in_=convT[:, cb, :], func=AF.Copy),
                      reads=[f"conv{cb}"], writes=[f"yb{cb % 2}"])
                P.act(lambda e, cb=cb, ysq=ysq: e.activation(out=ysq, in_=convT[:, cb, :], func=AF.Square),
                      reads=[f"conv{cb}"], writes=[f"ysq{cb % 2}"])
                P.pe(lambda e, cb=cb, yb=yb: e.matmul(bank(4), lhsT=ones[:], rhs=yb, start=(cb == 0), stop=(cb == 11)),
                     reads=["ones", f"yb{cb % 2}"], writes=["ps4"])
                P.pe(lambda e, cb=cb, ysq=ysq: e.matmul(bank(5), lhsT=ones[:], rhs=ysq, start=(cb == 0),
                                                        stop=(cb == 11)),
                     reads=["ones", f"ysq{cb % 2}"], writes=["ps5"])
            P.act(lambda e: e.activation(out=MEAN, in_=bank(4), func=AF.Copy, scale=1.0 / BW),
                  reads=["ps4"], writes=["MEAN"])
            P.dve(lambda e: e.tensor_tensor(out=VAR, in0=MEAN, in1=MEAN, op=ALU.mult), reads=["MEAN"], writes=["VAR"])
            P.dve(lambda e: e.scalar_tensor_tensor(out=RSTD, in0=bank(5), scalar=1.0 / BW, in1=VAR,
                                                   op0=ALU.mult, op1=ALU.subtract),
                  reads=["ps5", "VAR"], writes=["RSTD"])
            P.dve(lambda e: e.tensor_scalar(out=VAR, in0=RSTD, scalar1=EPS, scalar2=None, op0=ALU.add),
                  reads=["RSTD"], writes=["VAR"])
            P.dve(lambda e: e.reciprocal(out=VAR, in_=VAR), reads=["VAR"], writes=["VAR"])
            P.act(lambda e: e.activation(out=RSTD, in_=VAR, func=AF.Sqrt), reads=["VAR"], writes=["RSTD"])
            P.dve(lambda e: e.scalar_tensor_tensor(out=NMR, in0=MEAN, scalar=-1.0, in1=RSTD, op0=ALU.mult, op1=ALU.mult),
                  reads=["MEAN", "RSTD"], writes=["NMR"])
            for cb in range(12):
                z = zb[cb % 2]
                a = ab[cb % 2]
                P.dve(lambda e, cb=cb, z=z: e.tensor_tensor(out=z, in0=convT[:, cb, :], in1=RSTD, op=ALU.mult),
                      reads=[f"conv{cb}", "RSTD"], writes=[f"z{cb % 2}"])
                P.dve(lambda e, z=z: e.tensor_tensor(out=z, in0=z, in1=NMR, op=ALU.add),
                      reads=[f"z{cb % 2}", "NMR"], writes=[f"z{cb % 2}"])
                P.act(lambda e, cb=cb, z=z, a=a: e.activation(out=a, in_=z, func=AF.Silu, scale=lng[:, cb:cb + 1],
                                                             bias=lnb[:, cb:cb + 1]),
                      reads=[f"z{cb % 2}", "lng", "lnb"], writes=[f"a{cb % 2}"])
                P.pool(lambda e, cb=cb, a=a: e.tensor_tensor(out=yT[:, cb, :], in0=a, in1=yT[:, cb, :], op=ALU.mult),
                       reads=[f"a{cb % 2}", f"yT{cb}"], writes=[f"yT{cb}"])
            wo_prefetch(1)
            mem_attn()
            out_proj(1, tok0, last_layer=True)

        for s in range(nseq):
            mem_kv(s)
            for gs in range(ngs):
                gi = s * ngs + gs
                tok0 = s * seqlen + gs * T
                for c in range(NCH):
                    P.dma("sp", f"xl{c}", lambda e, c=c, tok0=tok0: e.dma_start(
                        out=X[:, c, :], in_=x_d[tok0 + c * 128: tok0 + (c + 1) * 128, :]), writes=[f"X{c}"])
                P.dma("sp", "ropeld", lambda e, gi=gi: e.dma_start(
                    out=rope[:], in_=rope_s[:, :, gi * NCH:(gi + 1) * NCH, :].rearrange("w p c d -> p w c d")),
                    reads=[f"rope_s{gi}"], writes=["rope", "rope0", "rope1", "rope2"])
                if 0 in layers:
                    layer0(gi, gs, tok0)
                if 1 in layers:
                    layer1(gi, gs, tok0)
        with nc.allow_low_precision("bf16 matmul operands, fp32 accumulation"):
            P.finish()
        build_program.last_prog = P
    return nc


def _core_inputs(i, nseq, inputs, consts):
    f = np.float32
    sl = slice(i * nseq, (i + 1) * nseq)
    x = np.ascontiguousarray(inputs["x"][sl]).reshape(-1, D)
    mem = np.ascontiguousarray(inputs["mem"][sl]).reshape(-1, D)
    pos = np.ascontiguousarray(inputs["positions"][sl]).reshape(-1)
    ncht = pos.shape[0] // 128
    m = {
        "x": x, "mem": mem,
        "pos": np.ascontiguousarray(pos.reshape(ncht, 128).T).astype(np.int32),
        "w_mem_kv": inputs["w_mem_kv"],
        "ret_w_in": inputs["ret_w_in"][0], "conv_w_in": inputs["conv_w_in"][0],
        "ret_w_out": inputs["ret_w_out"][0], "conv_w_out": inputs["conv_w_out"][0],
        "g_mem": np.ascontiguousarray(inputs["mem_norm_g"].reshape(8, 128).T),
        "g_pre": np.ascontiguousarray(inputs["norm_pre_g"].reshape(2, 8, 128).transpose(2, 0, 1)),
        "g_post": np.ascontiguousarray(np.broadcast_to(inputs["norm_post_g"][None], (128, 2, D))),
        "dw_w": np.ascontiguousarray(inputs["conv_dw_w"][0].reshape(CW, 12, 128).transpose(2, 1, 0)),
        "dw_b": np.ascontiguousarray(inputs["conv_dw_b"][0].reshape(12, 128).T),
        "ln_g": np.ascontiguousarray(inputs["conv_ln_g"][0].reshape(12, 128).T),
        "ln_b": np.ascontiguousarray(inputs["conv_ln_b"][0].reshape(12, 128).T),
    }
    m.update(consts)
    return {k: np.ascontiguousarray(v) for k, v in m.items()}


_CACHE = {}


def run(inputs, n_cores, nseq, seqlen, layers=(0, 1), trace=False):
    key = (nseq, seqlen, tuple(layers))
    if key not in _CACHE:
        _CACHE[key] = build_program(nseq, seqlen, layers)
    nc = _CACHE[key]
    consts, _ = make_consts()
    inputs = {k: np.asarray(v) for k, v in inputs.items()}
    in_maps = [_core_inputs(i, nseq, inputs, consts) for i in range(n_cores)]
    res = run_bass_kernel_spmd(nc, in_maps, core_ids=list(range(n_cores)), **({"trace": True} if trace else {}))
    out = np.stack([r["out"].reshape(nseq, seqlen, D) for r in res.results], axis=0)
    return out.reshape(n_cores * nseq, seqlen, D), res


def kernel(x, mem, positions, mem_norm_g, w_mem_kv, norm_pre_g, norm_post_g, ret_w_in, ret_w_out,
           conv_w_in, conv_dw_w, conv_dw_b, conv_ln_g, conv_ln_b, conv_w_out):
    inputs = dict(x=x, mem=mem, positions=positions, mem_norm_g=mem_norm_g, w_mem_kv=w_mem_kv,
                  norm_pre_g=norm_pre_g, norm_post_g=norm_post_g, ret_w_in=ret_w_in, ret_w_out=ret_w_out,
                  conv_w_in=conv_w_in, conv_dw_w=conv_dw_w, conv_dw_b=conv_dw_b, conv_ln_g=conv_ln_g,
                  conv_ln_b=conv_ln_b, conv_w_out=conv_w_out)
    out, _ = run(inputs, 8, 2, 2048)
    return out.astype(np.float32)
```
